# Optimizing a Trainium2 kernel written in Bass

```python
import math
import jax, jax.numpy as jnp
from jax import lax
import numpy as np

D_MODEL = 1024
BATCH = 8
SEQ = 8192
DEPTH = 2

GRID_W = 64
CTX_LEN = 256
RMS_EPS = 1e-6
ROPE_BASE = 10000.0
N_MOD = 6
RET_HEADS = 4
RET_DK = 64
RET_DV = 128
RET_CHUNK = 128
CONV_WIDTH = 512
MLA_HEADS = 8
MLA_Q_RANK = 384
MLA_KV_RANK = 256
MLA_NOPE = 64
MLA_ROPE = 32
MLA_V = 64
ATTN_BLOCK = 128
N_BRANCH = 3
N_EXPERTS = 16
N_GROUPS = 4
EXPERTS_PER_GROUP = N_EXPERTS // N_GROUPS
TOP_K = 2
D_EXPERT = 512
IN_SIZES = (RET_HEADS * RET_DK, RET_HEADS * RET_DK, RET_HEADS * RET_DV, RET_HEADS * RET_DV,
            CONV_WIDTH, CONV_WIDTH, CONV_WIDTH, MLA_Q_RANK, MLA_KV_RANK, MLA_ROPE, N_BRANCH * D_MODEL)
IN_COLS = sum(IN_SIZES)

kernel_name = "hybrid_retention_conv_mla_moe_dit"


def split_cols(p):
    out = []
    off = 0
    for s in IN_SIZES:
        out.append(p[..., off:off + s])
        off += s
    return out


def rms_norm(x, w):
    xf = x.astype(jnp.float32)
    y = xf * lax.rsqrt(jnp.mean(xf * xf, axis=-1, keepdims=True) + RMS_EPS)
    return (y * w.astype(jnp.float32)).astype(x.dtype)


def rotary(x, pos):
    half = x.shape[-1] // 2
    inv = ROPE_BASE ** (-jnp.arange(half, dtype=jnp.float32) / half)
    ang = pos.astype(jnp.float32)[:, None] * inv[None, :]
    cos = jnp.cos(ang)[None, :, None, :]
    sin = jnp.sin(ang)[None, :, None, :]
    xf = x.astype(jnp.float32)
    x1, x2 = xf[..., :half], xf[..., half:]
    return jnp.concatenate([x1 * cos - x2 * sin, x2 * cos + x1 * sin], axis=-1).astype(x.dtype)


def axial_rotary(x, rows, cols):
    half = x.shape[-1] // 2
    return jnp.concatenate([rotary(x[..., :half], rows), rotary(x[..., half:], cols)], axis=-1)


def ret_split(p, dim, pos):
    b, l, _ = p.shape
    t = p.reshape(b, l, RET_HEADS, dim)
    if pos is not None:
        t = rotary(t, pos)
    return t.astype(jnp.float32)


def retention_chunked(q, k, v, log_g, s0, strict):
    b, l, h, dk = q.shape
    dv = v.shape[-1]
    n = l // RET_CHUNK
    qc = q.reshape(b, n, RET_CHUNK, h, dk)
    kc = k.reshape(b, n, RET_CHUNK, h, dk)
    vc = v.reshape(b, n, RET_CHUNK, h, dv)
    idx = jnp.arange(RET_CHUNK, dtype=jnp.float32)
    rel = idx[:, None] - idx[None, :]
    keep = (rel > 0) if strict else (rel >= 0)
    decay = jnp.where(keep[None], jnp.exp(log_g[:, None, None] * jnp.maximum(rel, 0.0)[None]), 0.0)
    scores = jnp.einsum('bnihd,bnjhd->bnhij', qc, kc) * decay[None, None]
    inner = jnp.einsum('bnhij,bnjhe->bnihe', scores, vc)
    zeta = jnp.exp(log_g[None, :] * (RET_CHUNK - 1 - idx)[:, None])
    u = jnp.einsum('bnjhd,bnjhe->nbhde', kc * zeta[None, None, :, :, None], vc)
    chunk_decay = jnp.exp(log_g * RET_CHUNK)[None, :, None, None]

    def step(s, u_n):
        return chunk_decay * s + u_n, s

    _, s_prev = lax.scan(step, s0, u)
    xi = jnp.exp(log_g[None, :] * (idx + 1.0)[:, None])
    cross = jnp.einsum('bnihd,nbhde->bnihe', qc * xi[None, None, :, :, None], s_prev)
    return (inner + cross).reshape(b, l, h, dv)


def ret_final_state(k, v, log_g, reverse):
    l = k.shape[1]
    pos = jnp.arange(l, dtype=jnp.float32)
    dist = pos if reverse else (l - 1.0 - pos)
    w = jnp.exp(dist[:, None] * log_g[None, :])
    return jnp.einsum('blhd,blhe->bhde', k * w[None, :, :, None], v)


def retention_bidir(q, k, v, log_gf, log_gb, s_f, s_b):
    y_f = retention_chunked(q, k, v, log_gf, s_f, False)
    y_b = retention_chunked(jnp.flip(q, 1), jnp.flip(k, 1), jnp.flip(v, 1), log_gb, s_b, True)
    return y_f + jnp.flip(y_b, 1)


def ret_output(y, pg, gn_w, w_o):
    b, l = y.shape[:2]
    yn = y * lax.rsqrt(jnp.mean(y * y, axis=-1, keepdims=True) + RMS_EPS)
    yn = yn.reshape(b, l, -1) * gn_w.astype(jnp.float32)
    return (jax.nn.silu(pg) * yn.astype(pg.dtype)) @ w_o


def short_conv(pb, pc, px, conv_w):
    u = pc * px
    up = jnp.pad(u, ((0, 0), (1, 1), (0, 0)))
    y = up[:, :-2] * conv_w[:, 0] + up[:, 1:-1] * conv_w[:, 1] + up[:, 2:] * conv_w[:, 2]
    return pb * y


def mla_queries(pq, q_norm, w_uq, rows, cols):
    b, l, _ = pq.shape
    q = (rms_norm(pq, q_norm) @ w_uq).reshape(b, l, MLA_HEADS, MLA_NOPE + MLA_ROPE)
    qn, qr = q[..., :MLA_NOPE], q[..., MLA_NOPE:]
    if rows is not None:
        qr = axial_rotary(qr, rows, cols)
    return qn, qr


def mla_keys(pkv, pkr, kv_norm, w_ukv, rows, cols):
    b, l, _ = pkv.shape
    kv = (rms_norm(pkv, kv_norm) @ w_ukv).reshape(b, l, MLA_HEADS, MLA_NOPE + MLA_V)
    kn, v = kv[..., :MLA_NOPE], kv[..., MLA_NOPE:]
    kr = pkr.reshape(b, l, 1, MLA_ROPE)
    if rows is not None:
        kr = axial_rotary(kr, rows, cols)
    return kn, kr, v


def mla_attend(qn, qr, kn, kr, v):
    scale = (MLA_NOPE + MLA_ROPE) ** -0.5
    s = jnp.einsum('bqhd,bkhd->bhqk', qn, kn) + jnp.einsum('bqhr,bkr->bhqk', qr, kr[:, :, 0])
    p = jax.nn.softmax(s.astype(jnp.float32) * scale, axis=-1).astype(v.dtype)
    o = jnp.einsum('bhqk,bkhd->bqhd', p, v)
    return o.reshape(o.shape[0], o.shape[1], -1)


def mla_attend_blocked(qn, qr, kn, kr, v):
    b, l = qn.shape[:2]
    nb = l // ATTN_BLOCK

    def to_blocks(t):
        return t.reshape(b, nb, ATTN_BLOCK, *t.shape[2:]).swapaxes(0, 1)

    out = lax.map(lambda qs: mla_attend(qs[0], qs[1], kn, kr, v), (to_blocks(qn), to_blocks(qr)))
    return out.swapaxes(0, 1).reshape(b, l, -1)


def merge_branches(pgate, y_ret, y_conv, y_mla, w_out):
    g = jax.nn.sigmoid(pgate.reshape(*pgate.shape[:-1], N_BRANCH, D_MODEL))
    m = g[..., 0, :] * y_ret + g[..., 1, :] * y_conv + g[..., 2, :] * y_mla
    return m @ w_out


def moe(h, w_router, router_bias, w1, w3, w2):
    shp = h.shape
    t = h.reshape(-1, shp[-1])
    scores = jax.nn.sigmoid((t @ w_router).astype(jnp.float32))
    biased = scores + router_bias.astype(jnp.float32)
    grp = biased.reshape(-1, N_GROUPS, EXPERTS_PER_GROUP)
    group_score = jnp.sum(lax.top_k(grp, TOP_K)[0], axis=-1)
    g_sel = jnp.argmax(group_score, axis=-1)
    in_grp = jnp.take_along_axis(grp, g_sel[:, None, None], axis=1)[:, 0]
    idx = g_sel[:, None] * EXPERTS_PER_GROUP + lax.top_k(in_grp, TOP_K)[1]
    w = jnp.take_along_axis(scores, idx, axis=1)
    w = w / jnp.sum(w, axis=-1, keepdims=True)
    gate = jnp.sum(jax.nn.one_hot(idx, N_EXPERTS, dtype=jnp.float32) * w[..., None], axis=1).astype(h.dtype)
    out = jnp.zeros_like(t)
    for e in range(N_EXPERTS):
        he = (jax.nn.silu(t @ w1[e]) * (t @ w3[e])) @ w2[e]
        out = out + gate[:, e:e + 1] * he
    return out.reshape(shp)


def trunk_layer(x, xc, silu_c, silu_cc, rows, cols, pos, w_ada, b_ada, norm1, norm2, w_in,
                ret_decay, ret_gn, w_ret_o, conv_w, w_conv_o, mla_q_norm, w_uq, mla_kv_norm,
                w_ukv, w_mla_o, w_out, w_router, router_bias, w1, w3, w2, update_ctx):
    mod_x = jnp.split((silu_c @ w_ada + b_ada)[:, None, :], N_MOD, axis=-1)
    mod_c = jnp.split((silu_cc @ w_ada + b_ada)[None, None, :], N_MOD, axis=-1)
    hx = rms_norm(x, norm1) * (1.0 + mod_x[1]) + mod_x[0]
    hc = rms_norm(xc, norm1) * (1.0 + mod_c[1]) + mod_c[0]
    px = split_cols(hx @ w_in)
    pc = split_cols(hc @ w_in)
    log_gf = jax.nn.log_sigmoid(ret_decay[0].astype(jnp.float32))
    log_gb = jax.nn.log_sigmoid(ret_decay[1].astype(jnp.float32))

    kc = ret_split(pc[1], RET_DK, None) * (RET_DK ** -0.5)
    vc = ret_split(pc[2], RET_DV, None)
    s_f = ret_final_state(kc, vc, log_gf, False)
    s_b = ret_final_state(kc, vc, log_gb, True)
    qx = ret_split(px[0], RET_DK, pos)
    kx = ret_split(px[1], RET_DK, pos) * (RET_DK ** -0.5)
    vx = ret_split(px[2], RET_DV, None)
    y_ret = ret_output(retention_bidir(qx, kx, vx, log_gf, log_gb, s_f, s_b), px[3], ret_gn, w_ret_o)

    y_conv = short_conv(px[4], px[5], px[6], conv_w) @ w_conv_o

    kn_c, kr_c, v_c = mla_keys(pc[8], pc[9], mla_kv_norm, w_ukv, None, None)
    kn_x, kr_x, v_x = mla_keys(px[8], px[9], mla_kv_norm, w_ukv, rows, cols)
    qn_x, qr_x = mla_queries(px[7], mla_q_norm, w_uq, rows, cols)
    kn = jnp.concatenate([kn_c, kn_x], axis=1)
    kr = jnp.concatenate([kr_c, kr_x], axis=1)
    v = jnp.concatenate([v_c, v_x], axis=1)
    y_mla = mla_attend_blocked(qn_x, qr_x, kn, kr, v) @ w_mla_o

    x_mid = x + mod_x[2] * merge_branches(px[10], y_ret, y_conv, y_mla, w_out)
    h2x = rms_norm(x_mid, norm2) * (1.0 + mod_x[4]) + mod_x[3]

    if update_ctx:
        zero = jnp.zeros_like(s_f)
        qc = ret_split(pc[0], RET_DK, None)
        yc_ret = ret_output(retention_bidir(qc, kc, vc, log_gf, log_gb, zero, zero), pc[3], ret_gn, w_ret_o)
        yc_conv = short_conv(pc[4], pc[5], pc[6], conv_w) @ w_conv_o
        qn_c, qr_c = mla_queries(pc[7], mla_q_norm, w_uq, None, None)
        yc_mla = mla_attend(qn_c, qr_c, kn_c, kr_c, v_c) @ w_mla_o
        xc_mid = xc + mod_c[2] * merge_branches(pc[10], yc_ret, yc_conv, yc_mla, w_out)
        h2c = rms_norm(xc_mid, norm2) * (1.0 + mod_c[4]) + mod_c[3]
        n_ctx = xc.shape[1]
        f = moe(jnp.concatenate([h2c, h2x], axis=1), w_router, router_bias, w1, w3, w2)
        return x_mid + mod_x[5] * f[:, n_ctx:], xc_mid + mod_c[5] * f[:, :n_ctx]

    return x_mid + mod_x[5] * moe(h2x, w_router, router_bias, w1, w3, w2), xc


def setup_inputs(seed: int = 0) -> dict:
    key = jax.random.key(seed)
    ks = jax.random.split(key, 32)
    f32 = jnp.float32

    def nrm(k, shape, scale):
        return jax.random.normal(k, shape, f32) * scale

    base_decay = jnp.log(2.0 ** (5.0 + jnp.arange(RET_HEADS, dtype=f32)) - 1.0)
    return {
        'x': nrm(ks[0], (BATCH, SEQ, D_MODEL), 1.0),
        'c': nrm(ks[1], (BATCH, D_MODEL), 1.0),
        'ctx': nrm(ks[2], (BATCH, CTX_LEN, D_MODEL), 1.0),
        'c_ctx': nrm(ks[3], (D_MODEL,), 1.0),
        'w_ada': nrm(ks[4], (DEPTH, D_MODEL, N_MOD * D_MODEL), 0.5 * D_MODEL ** -0.5),
        'b_ada': nrm(ks[5], (DEPTH, N_MOD * D_MODEL), 0.02),
        'norm1': 1.0 + nrm(ks[6], (DEPTH, D_MODEL), 0.05),
        'norm2': 1.0 + nrm(ks[7], (DEPTH, D_MODEL), 0.05),
        'w_in': nrm(ks[8], (DEPTH, D_MODEL, IN_COLS), D_MODEL ** -0.5),
        'ret_decay': base_decay[None, None, :] + nrm(ks[9], (DEPTH, 2, RET_HEADS), 0.1),
        'ret_gn': 1.0 + nrm(ks[10], (DEPTH, RET_HEADS * RET_DV), 0.05),
        'w_ret_o': nrm(ks[11], (DEPTH, RET_HEADS * RET_DV, D_MODEL), (RET_HEADS * RET_DV) ** -0.5),
        'conv_w': nrm(ks[12], (DEPTH, CONV_WIDTH, 3), 3.0 ** -0.5),
        'w_conv_o': nrm(ks[13], (DEPTH, CONV_WIDTH, D_MODEL), CONV_WIDTH ** -0.5),
        'mla_q_norm': 1.0 + nrm(ks[14], (DEPTH, MLA_Q_RANK), 0.05),
        'w_uq': nrm(ks[15], (DEPTH, MLA_Q_RANK, MLA_HEADS * (MLA_NOPE + MLA_ROPE)), MLA_Q_RANK ** -0.5),
        'mla_kv_norm': 1.0 + nrm(ks[16], (DEPTH, MLA_KV_RANK), 0.05),
        'w_ukv': nrm(ks[17], (DEPTH, MLA_KV_RANK, MLA_HEADS * (MLA_NOPE + MLA_V)), MLA_KV_RANK ** -0.5),
        'w_mla_o': nrm(ks[18], (DEPTH, MLA_HEADS * MLA_V, D_MODEL), (MLA_HEADS * MLA_V) ** -0.5),
        'w_out': nrm(ks[19], (DEPTH, D_MODEL, D_MODEL), D_MODEL ** -0.5),
        'w_router': nrm(ks[20], (D_MODEL, N_EXPERTS), D_MODEL ** -0.5),
        'router_bias': nrm(ks[21], (N_EXPERTS,), 0.01),
        'w1': nrm(ks[22], (DEPTH, N_EXPERTS, D_MODEL, D_EXPERT), D_MODEL ** -0.5),
        'w3': nrm(ks[23], (DEPTH, N_EXPERTS, D_MODEL, D_EXPERT), D_MODEL ** -0.5),
        'w2': nrm(ks[24], (DEPTH, N_EXPERTS, D_EXPERT, D_MODEL), D_EXPERT ** -0.5),
        'final_norm': 1.0 + nrm(ks[25], (D_MODEL,), 0.05),
    }


def reference(x, c, ctx, c_ctx, w_ada, b_ada, norm1, norm2, w_in, ret_decay, ret_gn, w_ret_o,
              conv_w, w_conv_o, mla_q_norm, w_uq, mla_kv_norm, w_ukv, w_mla_o, w_out,
              w_router, router_bias, w1, w3, w2, final_norm):
    n_tok = x.shape[1]
    n_rows = n_tok // GRID_W
    pos = jnp.arange(n_tok, dtype=jnp.int32)
    rows = jnp.repeat(jnp.arange(n_rows, dtype=jnp.int32), GRID_W)
    cols = pos - rows * GRID_W
    silu_c = jax.nn.silu(c)
    silu_cc = jax.nn.silu(c_ctx)
    xc = ctx
    for l in range(DEPTH):
        x, xc = trunk_layer(x, xc, silu_c, silu_cc, rows, cols, pos, w_ada[l], b_ada[l], norm1[l], norm2[l],
                            w_in[l], ret_decay[l], ret_gn[l], w_ret_o[l], conv_w[l], w_conv_o[l],
                            mla_q_norm[l], w_uq[l], mla_kv_norm[l], w_ukv[l], w_mla_o[l], w_out[l],
                            w_router, router_bias, w1[l], w3[l], w2[l], l < DEPTH - 1)
    return rms_norm(x, final_norm)
```

```python
import contextlib
import numpy as np
import concourse.bass as bass
import concourse.mybir as mybir
from concourse.bass_utils import run_bass_kernel_spmd

F32 = mybir.dt.float32
BF16 = mybir.dt.bfloat16
AF = mybir.ActivationFunctionType
ALU = mybir.AluOpType
AX = mybir.AxisListType

D = 1024
DEPTH = 2
CTX = 256
EPS = 1e-6
NE = 16
import os as _os
CUT = int(_os.environ.get("KCUT", "0"))
CA = dict(RQ=0, RQS=256, RK=512, RKS=768, RV=1024, PG=1536, CB=2048, CC=2560, CX=3072)
NCA = 3584
CBk = dict(QD=0, KVD=384, KR=640, KRS=672, GT=704)
NCB = 704 + 3072


class Buf:
    __slots__ = ("name", "w", "r", "t", "chan", "psum")

    def __init__(self, name, t=None):
        self.name = name
        self.w = {}
        self.r = {}
        self.t = t
        self.chan = None
        self.psum = False

    def __getitem__(self, k):
        return self.t[k]


class Chan:
    __slots__ = ("sem", "cnt")

    def __init__(self, sem):
        self.sem = sem
        self.cnt = 0


class KB:
    ENGS = ("pe", "act", "dve", "pool", "sp")

    def __init__(self, nc, stack, nchan=72, nsw=24):
        self.nc = nc
        self.gst = stack
        self.st = stack
        self.eng = {"pe": nc.tensor, "act": nc.scalar, "dve": nc.vector, "pool": nc.gpsimd, "sp": nc.sync}
        self.sem = {e: stack.enter_context(nc.semaphore("s_" + e)) for e in self.ENGS}
        self.cnt = {e: 0 for e in self.ENGS}
        self.seen = {e: {} for e in self.ENGS}
        self.chans = [Chan(stack.enter_context(nc.semaphore("c%d" % i))) for i in range(nchan)]
        self.swchans = [Chan(stack.enter_context(nc.semaphore("w%d" % i))) for i in range(nsw)]
        self.nextchan = 0
        self.nextsw = 0
        self.maxwait = {}
        self.nwait = 0
        self.nins = 0
        self.uid = 0

    def sb(self, name, shape, dt):
        self.uid += 1
        t = self.st.enter_context(self.nc.sbuf_tensor("%s_%d" % (name, self.uid), list(shape), dt))
        return Buf(name, t)

    def ps(self, name, shape, dt):
        self.uid += 1
        t = self.st.enter_context(self.nc.psum_tensor("%s_%d" % (name, self.uid), list(shape), dt))
        b = Buf(name, t)
        b.psum = True
        return b

    def dram(self, name, shape, dt, kind="Internal"):
        t = self.nc.dram_tensor(name, list(shape), dt, kind=kind)
        return Buf(name, t.ap())

    def chan(self, q="sp"):
        if q == "pool":
            c = self.swchans[self.nextsw]
            self.nextsw += 1
            return c
        c = self.chans[self.nextchan]
        self.nextchan += 1
        return c

    def bchan(self, b, q):
        if b.chan is None:
            b.chan = {}
        key = "sw" if q == "pool" else "hw"
        if key not in b.chan:
            b.chan[key] = self.chan(q)
        return b.chan[key]

    def phase_begin(self, stack):
        self.st = stack
        self.nextchan = 0
        self.nextsw = 0

    def _wait(self, e, need):
        seen = self.seen[e]
        eng = self.eng[e]
        for sem, val in need.items():
            if seen.get(sem, 0) >= val:
                continue
            eng.wait_ge(sem, val)
            seen[sem] = val
            self.nwait += 1
            if self.maxwait.get(sem, 0) < val:
                self.maxwait[sem] = val

    def op(self, e, fn, reads=(), writes=(), inc=True):
        mysem = self.sem[e]
        need = {}
        for b in reads:
            for s, v in b.w.items():
                if need.get(s, 0) < v:
                    need[s] = v
            if b.psum:
                for s, v in b.r.items():
                    if s is not mysem and need.get(s, 0) < v:
                        need[s] = v
        waw_self = (e != "pe")
        for b in writes:
            for s, v in b.w.items():
                if (s is not mysem or waw_self) and need.get(s, 0) < v:
                    need[s] = v
            for s, v in b.r.items():
                if (s is not mysem or waw_self) and need.get(s, 0) < v:
                    need[s] = v
        if need:
            self._wait(e, need)
        ins = fn()
        self.nins += 1
        if inc:
            self.cnt[e] += 1
            ins.then_inc(mysem, 1)
            tok = self.cnt[e]
        else:
            tok = self.cnt[e] + 1
        for b in reads:
            if b.r.get(mysem, 0) < tok:
                b.r[mysem] = tok
        for b in writes:
            b.w[mysem] = tok
        return ins

    def dma(self, q, out, in_, reads=(), writes=(), chan=None, **kw):
        need = {}
        for b in reads:
            for s, v in b.w.items():
                if need.get(s, 0) < v:
                    need[s] = v
        for b in writes:
            for s, v in b.w.items():
                if need.get(s, 0) < v:
                    need[s] = v
            for s, v in b.r.items():
                if need.get(s, 0) < v:
                    need[s] = v
        if need:
            self._wait(q, need)
        idx = kw.pop("idx", None)
        if idx is not None:
            mode, iap = idx
            off = bass.IndirectOffsetOnAxis(ap=iap, axis=0)
            if mode == "gather":
                ins = self.nc.gpsimd.indirect_dma_start(out=out, out_offset=None, in_=in_, in_offset=off, **kw)
            else:
                ins = self.nc.gpsimd.indirect_dma_start(out=out, out_offset=off, in_=in_, in_offset=None, **kw)
        else:
            ins = self.eng[q].dma_start(out=out, in_=in_, **kw)
        self.nins += 1
        chan.cnt += 16
        ins.then_inc(chan.sem, 16)
        tok = chan.cnt
        for b in reads:
            if b.r.get(chan.sem, 0) < tok:
                b.r[chan.sem] = tok
        for b in writes:
            b.w[chan.sem] = tok
        return ins

    def load(self, q, dst, dst_ap, src_ap, src=None, chan=None, **kw):
        if chan is None:
            chan = self.bchan(dst, q)
        return self.dma(q, dst_ap, src_ap, reads=([src] if src is not None else []), writes=[dst], chan=chan, **kw)

    def store(self, q, dst, dst_ap, src, src_ap, **kw):
        return self.dma(q, dst_ap, src_ap, reads=[src], writes=[dst], chan=self.bchan(src, q), **kw)

    def barrier(self):
        need = {self.sem[e]: self.cnt[e] for e in self.ENGS if self.cnt[e] > 0}
        for c in self.chans + self.swchans + getattr(self, "extra_chans", []):
            if c.cnt > 0:
                need[c.sem] = c.cnt
        for e in self.ENGS:
            n2 = {s: v for s, v in need.items() if s is not self.sem[e]}
            self._wait(e, n2)

    def mm(self, ob, o, lb, l, rb, r, start, stop, inc=None):
        nc = self.nc
        return self.op("pe", lambda: nc.tensor.matmul(o, l, r, start=start, stop=stop),
                       reads=[lb, rb], writes=[ob], inc=(stop if inc is None else inc))

    def tr(self, ob, o, ib, i, idb, idap, inc=True):
        nc = self.nc
        return self.op("pe", lambda: nc.tensor.transpose(o, i, idap), reads=[ib, idb], writes=[ob], inc=inc)

    def tt(self, e, ob, o, ab, a, bb, b, op):
        eng = self.eng[e]
        return self.op(e, lambda: eng.tensor_tensor(out=o, in0=a, in1=b, op=op), reads=[ab, bb], writes=[ob])

    def ts(self, e, ob, o, ab, a, s1, s2, op0, op1=None, rd=(), accum=None, accb=None):
        eng = self.eng[e]
        kw = {}
        if op1 is not None:
            kw["op1"] = op1
        if accum is not None:
            kw["accum_out"] = accum
        wr = [ob] + ([accb] if accb is not None else [])
        return self.op(e, lambda: eng.tensor_scalar(out=o, in0=a, scalar1=s1, scalar2=s2, op0=op0, **kw),
                       reads=[ab] + list(rd), writes=wr)

    def stt(self, ob, o, ab, a, sc, bb, b, op0, op1, rd=(), accum=None, accb=None):
        nc = self.nc
        kw = {}
        if accum is not None:
            kw["accum_out"] = accum
        wr = [ob] + ([accb] if accb is not None else [])
        return self.op("dve", lambda: nc.vector.scalar_tensor_tensor(out=o, in0=a, scalar=sc, in1=b, op0=op0, op1=op1, **kw),
                       reads=[ab, bb] + list(rd), writes=wr)

    def act(self, ob, o, ib, i, func, rd=(), accb=None, **kw):
        nc = self.nc
        wr = [ob] + ([accb] if accb is not None else [])
        return self.op("act", lambda: nc.scalar.activation(out=o, in_=i, func=func, **kw), reads=[ib] + list(rd), writes=wr)

    def cp(self, e, ob, o, ib, i):
        if e == "act":
            nc = self.nc
            return self.op("act", lambda: nc.scalar.copy(out=o, in_=i), reads=[ib], writes=[ob])
        eng = self.eng[e]
        return self.op(e, lambda: eng.tensor_copy(out=o, in_=i), reads=[ib], writes=[ob])

    def memset(self, e, ob, o, v):
        eng = self.eng[e]
        return self.op(e, lambda: eng.memset(o, v), writes=[ob])


def _fm(v, nch):
    return np.ascontiguousarray(np.swapaxes(v.reshape(v.shape[:-1] + (nch, 128)), -1, -2))


def host_tables(NX):
    T = CTX + NX * 128
    S = NX * 128
    pos = np.arange(S, dtype=np.float32)
    inv = (np.float32(10000.0) ** (-np.arange(32, dtype=np.float32) / np.float32(32))).astype(np.float32)
    ang = (pos[None, :] * inv[:, None]).astype(np.float32)
    c = np.cos(ang).astype(np.float32)
    s = np.sin(ang).astype(np.float32)
    cos64 = np.concatenate([c, c], 0)
    sin64 = np.concatenate([-s, s], 0)
    tabr = np.zeros((2, 128, T), np.float32)
    tabr[0, :, :CTX] = 1.0
    tabr[0, :, CTX:] = np.concatenate([cos64, cos64], 0)
    tabr[1, :, CTX:] = np.concatenate([sin64, sin64], 0)
    rows = (np.arange(S) // 64).astype(np.float32)
    cols = (np.arange(S) % 64).astype(np.float32)
    inv8 = (np.float32(10000.0) ** (-np.arange(8, dtype=np.float32) / np.float32(8))).astype(np.float32)
    ar = (rows[None, :] * inv8[:, None]).astype(np.float32)
    ac = (cols[None, :] * inv8[:, None]).astype(np.float32)
    cos32 = np.concatenate([np.cos(ar), np.cos(ar), np.cos(ac), np.cos(ac)], 0).astype(np.float32)
    sin32 = np.concatenate([-np.sin(ar), np.sin(ar), -np.sin(ac), np.sin(ac)], 0).astype(np.float32)
    tabm = np.zeros((2, 96, T), np.float32)
    tabm[0, :, :CTX] = 1.0
    tabm[0, :64, CTX:] = 1.0
    tabm[0, 64:, CTX:] = cos32
    tabm[1, 64:, CTX:] = sin32
    i = np.arange(128, dtype=np.float32)
    relf = np.maximum(i[None, :] - i[:, None], 0.0)
    relb = np.maximum(i[:, None] - i[None, :], 0.0)
    mskf = (i[None, :] >= i[:, None]).astype(np.float32)
    mskb = (i[:, None] > i[None, :]).astype(np.float32)
    rconst = np.stack([relf, relb, mskf, mskb], 0).astype(np.float32)
    xi = np.stack([np.broadcast_to(i[None, :] + 1.0, (128, 128)),
                   np.broadcast_to(128.0 - i[None, :], (128, 128))], 0).astype(np.float32)
    zt = np.stack([127.0 - i, i], 1).astype(np.float32)
    ustr = (i[:, None] < i[None, :]).astype(np.float32)
    mc = np.zeros((128, 64), np.float32)
    mc[:, 0:9] = np.arange(9, dtype=np.float32)[None, :] * 1024.0
    mc[:, 16:16 + 16] = np.arange(16, dtype=np.float32)[None, :] * 1024.0
    mc[:, 40:44] = np.arange(4, dtype=np.float32)[None, :] * 128.0 + i[:, None]
    return dict(tabr=tabr, tabm=tabm, rconst=rconst, xicst=xi, ztcst=np.ascontiguousarray(zt), ustr=ustr, mcst=mc)


def host_weights(inp):
    L = DEPTH
    w_in = inp["w_in"]
    offs = np.cumsum([0, 256, 256, 512, 512, 512, 512, 512, 384, 256, 32, 3072])
    o_rq, o_rk, o_rv, o_pg, o_cb, o_cc, o_cx, o_qd, o_kvd, o_kr, o_gt = offs[:11]
    sw64 = np.concatenate([np.arange(32, 64), np.arange(0, 32)])
    swq = np.concatenate([h * 64 + sw64 for h in range(4)])
    sw16 = np.concatenate([np.arange(8, 16), np.arange(0, 8)])
    sw32 = np.concatenate([sw16, 16 + sw16])
    idxa = np.concatenate([o_rq + np.arange(256), o_rq + swq, o_rk + np.arange(256), o_rk + swq,
                           o_rv + np.arange(512), o_pg + np.arange(512), o_cb + np.arange(512),
                           o_cc + np.arange(512), o_cx + np.arange(512)])
    idxb = np.concatenate([o_qd + np.arange(384), o_kvd + np.arange(256), o_kr + np.arange(32), o_kr + sw32,
                           o_gt + np.arange(3072)])
    assert idxa.size == NCA and idxb.size == NCB
    out = {}
    out["w_ina"] = np.ascontiguousarray(w_in[:, :, idxa])
    out["w_inb"] = np.ascontiguousarray(w_in[:, :, idxb])
    w_uq = inp["w_uq"].reshape(L, 384, 8, 96)
    wq = np.zeros((L, 384, 8, 2, 96), np.float32)
    wq[:, :, :, 0, :] = w_uq
    wq[:, :, :, 1, 64:] = w_uq[:, :, :, 64 + sw32]
    out["w_uqe"] = wq.reshape(L, 384, 8 * 2 * 96)
    w_ukv = inp["w_ukv"].reshape(L, 256, 8, 128)
    out["w_ukn"] = np.ascontiguousarray(w_ukv[:, :, :, :64]).reshape(L, 256, 512)
    out["w_ukvv"] = np.ascontiguousarray(w_ukv[:, :, :, 64:]).reshape(L, 256, 512)
    out["norm1_t"] = _fm(inp["norm1"], 8)
    out["norm2_t"] = _fm(inp["norm2"], 8)
    out["ret_gn_t"] = _fm(inp["ret_gn"], 4)
    out["qn_t"] = _fm(inp["mla_q_norm"], 3)
    out["kvn_t"] = _fm(inp["mla_kv_norm"], 2)
    out["convw_t"] = np.ascontiguousarray(inp["conv_w"].reshape(L, 4, 128, 3).transpose(0, 2, 1, 3))
    out["ret_decay"] = np.ascontiguousarray(inp["ret_decay"].reshape(L, 1, 8))
    for nm in ("w_ada", "b_ada", "w_ret_o", "w_conv_o", "w_mla_o", "w_out", "w_router"):
        out[nm] = np.ascontiguousarray(inp[nm])
    out["w1r"] = np.ascontiguousarray(inp["w1"].reshape(L, NE, 8, 128, 512).transpose(0, 1, 3, 2, 4)).reshape(L, NE * 128, 4096)
    out["w3r"] = np.ascontiguousarray(inp["w3"].reshape(L, NE, 8, 128, 512).transpose(0, 1, 3, 2, 4)).reshape(L, NE * 128, 4096)
    out["w2r"] = np.ascontiguousarray(inp["w2"].reshape(L, NE, 4, 128, 1024).transpose(0, 1, 3, 2, 4)).reshape(L, NE * 128, 4096)
    out["norm2_r"] = np.ascontiguousarray(inp["norm2"].reshape(L, 1, D))
    out["router_bias"] = np.ascontiguousarray(inp["router_bias"].reshape(1, NE))
    out["final_norm"] = np.ascontiguousarray(inp["final_norm"].reshape(1, D))
    out["cc_t"] = _fm(inp["c_ctx"], 8)
    return out


def build(NX, dbg=(), stop_after=None):
    T = CTX + NX * 128
    NT = T // 128
    groups = [(0, CTX)] + [(CTX + 512 * i, 512) for i in range(NX // 4)]
    NG = len(groups)
    nc = bass.Bass("TRN2", target_bir_lowering=False)
    L = DEPTH

    def knd(name):
        return "ExternalOutput" if name in dbg else "Internal"

    with contextlib.ExitStack() as gst:
        k = KB(nc, gst)
        I = lambda name, shape, dt=F32: k.dram(name, shape, dt, kind="ExternalInput")
        x_in = I("x", [NX * 128, D]); ctx_in = I("ctx", [CTX, D])
        c_t = I("c_t", [128, 8]); cc_t = I("cc_t", [128, 8])
        w_ada = I("w_ada", [L, D, 6 * D]); b_ada = I("b_ada", [L, 6 * D])
        norm1_t = I("norm1_t", [L, 128, 8]); norm2_t = I("norm2_t", [L, 128, 8])
        w_ina = I("w_ina", [L, D, NCA]); w_inb = I("w_inb", [L, D, NCB])
        ret_decay = I("ret_decay", [L, 1, 8]); ret_gn_t = I("ret_gn_t", [L, 128, 4])
        w_ret_o = I("w_ret_o", [L, 512, D]); convw_t = I("convw_t", [L, 128, 4, 3]); w_conv_o = I("w_conv_o", [L, 512, D])
        qn_t = I("qn_t", [L, 128, 3]); w_uqe = I("w_uqe", [L, 384, 1536]); kvn_t = I("kvn_t", [L, 128, 2])
        w_ukn = I("w_ukn", [L, 256, 512]); w_ukvv = I("w_ukvv", [L, 256, 512])
        w_mla_o = I("w_mla_o", [L, 512, D]); w_out = I("w_out", [L, D, D])
        w_router = I("w_router", [D, NE]); router_bias = I("router_bias", [1, NE])
        w1r = I("w1r", [L, NE * 128, 4096]); w3r = I("w3r", [L, NE * 128, 4096]); w2r = I("w2r", [L, NE * 128, 4096])
        norm2_r = I("norm2_r", [L, 1, D]); ustr_in = I("ustr", [128, 128]); mcst_in = I("mcst", [128, 64])
        final_norm = I("final_norm", [1, D])
        tabr = I("tabr", [2, 128, T]); tabm = I("tabm", [2, 96, T])
        rconst = I("rconst", [4, 128, 128]); xicst = I("xicst", [2, 128, 128]); ztcst = I("ztcst", [128, 2])
        ident_in = I("ident", [128, 128])
        ybuf = k.dram("y", [NX * 128, D], F32, kind="ExternalOutput")
        y_out = ybuf.t

        class Scr:
            def __init__(self, name, shape, dt, tok_axis):
                self.ap = nc.dram_tensor(name, list(shape), dt, kind=knd(name)).ap()
                self.g = [Buf("%s_g%d" % (name, i), None) for i in range(NG)]
                self.tok_axis = tok_axis

        MOD = k.dram("MOD", [L, 2, 6 * D], F32, kind=knd("MOD"))
        HT = Scr("HT", [8, 128, T], BF16, 2)
        RQ = Scr("RQ", [4, 128, T], BF16, 2)
        RKZ = Scr("RKZ", [T, 2, 256], BF16, 0)
        RV = Scr("RV", [T, 512], BF16, 0)
        PGS = Scr("PGS", [4, 128, T], BF16, 2)
        UU = Scr("UU", [4, 128, T], BF16, 2)
        BBs = Scr("BBs", [4, 128, T], BF16, 2)
        QT = Scr("QT", [8, 96, T], BF16, 2)
        KN = Scr("KN", [8, 64, T], BF16, 2)
        KR = Scr("KR", [32, T], BF16, 1)
        VV = Scr("VV", [8, T, 65], BF16, 1)
        GT = Scr("GT", [24, 128, T], BF16, 2)
        YR = Scr("YR", [8, 128, T], BF16, 2)
        YC = Scr("YC", [8, 128, T], BF16, 2)
        OT = Scr("OT", [4, 128, T], BF16, 2)
        XM = Scr("XM", [T, D], F32, 0)
        XR = Scr("XR", [T, D], F32, 0)
        H2T = Scr("H2T", [8, 128, T], BF16, 2)
        GATE = Scr("GATE", [T, NE], F32, 0)
        NB = T // 1024 + 4
        NS = NB * 1024
        I32 = mybir.dt.int32
        H2R = Scr("H2R", [T, D], BF16, 0)
        H2S = k.dram("H2S", [NS, D], BF16, kind=knd("H2S"))
        G4S = k.dram("G4S", [NS, 16], F32, kind=knd("G4S"))
        YS = k.dram("YS", [NS, D], F32, kind=knd("YS"))
        xsrc0 = [Buf("xin_g%d" % i) for i in range(NG)]

        def gi_of_tile(t):
            return 0 if t < 2 else 1 + (t - 2) // 4

        def res_src(l, t):
            if l == 0:
                if t < 2:
                    return ctx_in[t * 128:(t + 1) * 128, :], xsrc0[0]
                return x_in[(t - 2) * 128:(t - 1) * 128, :], xsrc0[gi_of_tile(t)]
            return XR.ap[t * 128:(t + 1) * 128, :], XR.g[gi_of_tile(t)]

        ident32 = k.sb("ident32", [128, 128], F32)
        identb = k.sb("identb", [128, 128], BF16)
        onesb = k.sb("onesb", [128, 128], BF16)
        mhalf = k.sb("mhalf", [128, 512], F32)
        modT = k.sb("modT", [128, L, 48, 2], F32)
        vecs = k.sb("vecs", [128, L, 2, 4, 8], F32)
        n1t = k.sb("n1t", [128, L, 8], F32); n2t = k.sb("n2t", [128, L, 8], F32)
        gnt = k.sb("gnt", [128, L, 4], F32); qnt = k.sb("qnt", [128, L, 3], F32); kvnt = k.sb("kvnt", [128, L, 2], F32)
        cwt = k.sb("cwt", [128, L, 4, 3], F32)
        rbias = k.sb("rbias", [128, NE], F32)
        rdec = k.sb("rdec", [128, L, 8], F32)
        lg = k.sb("lg", [128, L, 8], F32)
        gC = k.sb("gC", [128, L, 8], F32)
        zt = k.sb("zt", [128, L, 8], F32)
        ztc = k.sb("ztc", [128, 2], F32)
        G16A = k.sb("G16A", [128, NT, 16], F32)
        GSLA = k.sb("GSLA", [128, NT, 4], F32)
        RANKA = k.sb("RANKA", [128, NT], F32)
        SLOTI = k.sb("SLOTI", [128, NT], mybir.dt.int32)
        IDXW = k.sb("IDXW", [128, T // 1024 + 4, 4], mybir.dt.int32)
        carry = k.sb("carry", [128, 4], F32)
        ustrb = k.sb("ustrb", [128, 128], BF16)
        mc = k.sb("mc", [128, 64], F32)
        initc = k.chans.pop()
        initsw = k.swchans.pop()
        k.extra_chans = [initc, initsw]
        initbufs = []

        def ld(dst, src, q="sp", dap=None):
            k.dma(q, dst[:] if dap is None else dap, src, writes=[dst], chan=(initsw if q == "pool" else initc))
            initbufs.append(dst)
        ld(ident32, ident_in[:, :])
        ld(identb, ident_in[:, :], q="pool")
        ld(n1t, norm1_t[:, :, :].rearrange("l p c -> p l c")); ld(n2t, norm2_t[:, :, :].rearrange("l p c -> p l c"))
        ld(gnt, ret_gn_t[:, :, :].rearrange("l p c -> p l c")); ld(qnt, qn_t[:, :, :].rearrange("l p c -> p l c"))
        ld(kvnt, kvn_t[:, :, :].rearrange("l p c -> p l c")); ld(cwt, convw_t[:, :, :, :].rearrange("l p c j -> p l c j"))
        ld(rbias, router_bias[0:1, :].partition_broadcast(128))
        for l in range(L):
            ld(rdec, ret_decay[l, 0:1, :].partition_broadcast(128), dap=rdec[:, l, :])
        ld(ztc, ztcst[:, :])
        ld(mc, mcst_in[:, :])
        ld(ustrb, ustr_in[:, :], q="pool")
        k.memset("pool", G16A, G16A[:], 0.0)
        for b_ in initbufs:
            for c_ in (initc, initsw):
                if c_.sem in b_.w:
                    b_.w[c_.sem] = c_.cnt
        k.memset("pool", onesb, onesb[:], 1.0)
        k.memset("pool", mhalf, mhalf[:], -0.5)
        k.act(lg, lg[:], rdec, rdec[:], AF.Exp, scale=-1.0)
        k.act(lg, lg[:], lg, lg[:], AF.Ln, bias=1.0)
        k.ts("dve", lg, lg[:], lg, lg[:], -1.0, None, ALU.mult)
        k.act(gC, gC[:], lg, lg[:], AF.Exp, scale=128.0)
        for l in range(L):
            for h in range(4):
                k.act(zt, zt[:, l, h:h + 1], ztc, ztc[:, 0:1], AF.Exp, rd=[lg], scale=lg[:, l, h:h + 1])
                k.act(zt, zt[:, l, 4 + h:5 + h], ztc, ztc[:, 1:2], AF.Exp, rd=[lg], scale=lg[:, l, 4 + h:5 + h])

        with contextlib.ExitStack() as pst:
            k.phase_begin(pst)
            ct = k.sb("ct", [128, 2, 8], F32)
            sc = k.sb("sc", [128, 8, 2], F32)
            wa = [k.sb("wa%d" % i, [128, 8, 512], F32) for i in range(4)]
            bb = k.sb("bb", [2, 6 * D], F32)
            modrow = k.sb("modrow", [2, 6 * D], F32)
            pm = [k.ps("pm%d" % i, [128, 512], F32) for i in range(4)]
            pt = k.ps("pt", [128, 96], F32)
            k.load("sp", ct, ct[:, 0, :], c_t[:, :])
            k.load("sp", ct, ct[:, 1, :], cc_t[:, :])
            for r in range(2):
                k.act(sc, sc[:, :, r], ct, ct[:, r, :], AF.Silu)
            it = 0
            for l in range(L):
                k.load("sp", bb, bb[:], b_ada[l:l + 1, :].partition_broadcast(2))
                for n in range(12):
                    s = it % 4
                    k.load("sp", wa[s], wa[s][:], w_ada[l, :, n * 512:(n + 1) * 512].rearrange("(kc p) n -> p kc n", p=128))
                    for kc in range(8):
                        k.mm(pm[s], pm[s][0:2, :], sc, sc[:, kc, :], wa[s], wa[s][:, kc, :], kc == 0, kc == 7)
                    k.tt("dve", modrow, modrow[:, n * 512:(n + 1) * 512], pm[s], pm[s][0:2, :], bb, bb[:, n * 512:(n + 1) * 512], ALU.add)
                    it += 1
                k.store("sp", MOD, MOD[l, :, :], modrow, modrow[:])
                for j in range(48):
                    k.tr(pt, pt[:, 2 * j:2 * j + 2], modrow, modrow[0:2, j * 128:(j + 1) * 128], ident32, ident32[0:2, 0:2], inc=(j == 47))
                k.cp("dve", modT, modT[:, l, :, :], pt, pt[:, :].rearrange("p (j r) -> p j r", r=2))
                for r in range(2):
                    mv = lambda kk: modT[:, l, kk * 8:(kk + 1) * 8, r]
                    k.stt(vecs, vecs[:, l, r, 0, :], modT, mv(1), 1.0, n1t, n1t[:, l, :], ALU.add, ALU.mult)
                    k.cp("dve", vecs, vecs[:, l, r, 1, :], modT, mv(0))
                    k.stt(vecs, vecs[:, l, r, 2, :], modT, mv(4), 1.0, n2t, n2t[:, l, :], ALU.add, ALU.mult)
                    k.cp("dve", vecs, vecs[:, l, r, 3, :], modT, mv(3))
            k.barrier()

        class RR:
            def __init__(self, bufs):
                self.bufs = bufs
                self.i = 0

            def nxt(self):
                b = self.bufs[self.i % len(self.bufs)]
                self.i += 1
                return b

        def rsqrt_chain(dst, dst_ap, src, src_ap, mult, add, tmp, tmp_ap, mh_ap):
            k.ts("dve", tmp, tmp_ap, src, src_ap, mult, add, ALU.mult, ALU.add)
            k.op("pool", lambda: nc.gpsimd.tensor_tensor(out=dst_ap, in0=tmp_ap, in1=mh_ap, op=ALU.pow),
                 reads=[tmp, mhalf], writes=[dst])

        def rsqrt_big(dst, dst_ap, src, src_ap, mult, add, tmp, tmp_ap):
            k.act(tmp, tmp_ap, src, src_ap, AF.Ln, scale=mult, bias=add)
            k.act(dst, dst_ap, tmp, tmp_ap, AF.Exp, scale=-0.5)

        for l in range(L):
            with contextlib.ExitStack() as pst:
                k.phase_begin(pst)
                Wa = k.sb("Wa", [128, 8, NCA], BF16)
                for i in range(NCA // 512):
                    k.load("pool", Wa, Wa[:, :, i * 512:(i + 1) * 512],
                           w_ina[l, :, i * 512:(i + 1) * 512].rearrange("(kc p) n -> p kc n", p=128))
                xin = [k.sb("xin%d" % i, [128, D], F32) for i in range(2)]
                junk = k.sb("junk", [128, D], BF16)
                xn = [k.sb("xn%d" % i, [128, D], BF16) for i in range(2)]
                st1s = [k.sb("st1_%d" % i, [128, 8], F32) for i in range(2)]
                hTm = k.sb("hTm", [128, 8, 128], F32)
                hT = [k.sb("hT%d" % i, [128, 8, 512], BF16) for i in range(2)]
                trc = [k.sb("trc%d" % i, [128, 512], F32) for i in range(2)]
                trs = [k.sb("trs%d" % i, [128, 512], F32) for i in range(2)]
                s1 = [k.sb("s1_%d" % i, [128, 512], F32) for i in range(2)]
                s2 = [k.sb("s2_%d" % i, [128, 512], F32) for i in range(2)]
                qk = [k.sb("qk%d" % i, [128, 4, 512], BF16) for i in range(2)]
                pgs = [k.sb("pgs%d" % i, [128, 4, 512], BF16) for i in range(2)]
                uu = [k.sb("uu%d" % i, [128, 4, 512], BF16) for i in range(2)]
                bbs = [k.sb("bbs%d" % i, [128, 4, 512], BF16) for i in range(2)]
                csb = [k.sb("csb%d" % i, [128, 512], F32) for i in range(2)]
                rvs = [k.sb("rvs%d" % i, [128, 4, 512], BF16) for i in range(2)]
                kz = [k.sb("kz%d" % i, [128, 4, 2, 256], BF16) for i in range(2)]
                pp = RR([k.ps("pp%d" % i, [128, 512], F32) for i in range(6)])
                ptr = k.ps("ptr", [128, 8, 128], BF16)
                ptk = k.ps("ptk", [128, 256], BF16)
                ti = 0
                for g, (t0, n) in enumerate(groups):
                    gs = g % 2
                    r = 1 if g == 0 else 0
                    ntile = n // 128
                    k.load("sp", trc[gs], trc[gs][:, 0:n], tabr[0, :, t0:t0 + n])
                    k.load("sp", trs[gs], trs[gs][:, 0:n], tabr[1, :, t0:t0 + n])
                    for j in range(ntile):
                        t = t0 // 128 + j
                        xs = ti % 2
                        ti += 1
                        sap, sbuf_ = res_src(l, t)
                        st1 = st1s[xs]
                        k.load("sp", xin[xs], xin[xs][:], sap, src=sbuf_)
                        k.act(junk, junk[:], xin[xs], xin[xs][:], AF.Square, accb=st1, accum_out=st1[:, 0:1])
                        rsqrt_chain(st1, st1[:, 2:3], st1, st1[:, 0:1], 1.0 / D, EPS, st1, st1[:, 1:2], mhalf[:, 0:1])
                        k.ts("dve", xn[xs], xn[xs][:], xin[xs], xin[xs][:], st1[:, 2:3], None, ALU.mult, rd=[st1])
                        for c in range(8):
                            k.tr(ptr, ptr[:, c, :], xn[xs], xn[xs][:, c * 128:(c + 1) * 128], identb, identb[:], inc=(c == 7))
                        k.tt("dve", hTm, hTm[:], ptr, ptr[:], vecs, vecs[:, l, r, 0, :].unsqueeze(2).to_broadcast([128, 8, 128]), ALU.mult)
                        k.tt("dve", hT[gs], hT[gs][:, :, j * 128:(j + 1) * 128], hTm, hTm[:],
                             vecs, vecs[:, l, r, 1, :].unsqueeze(2).to_broadcast([128, 8, 128]), ALU.add)
                    k.store("sp", HT.g[g], HT.ap[:, :, t0:t0 + n].rearrange("c p t -> p c t"), hT[gs], hT[gs][:, :, 0:n])

                    def proj(col0, M):
                        p = pp.nxt()
                        for kc in range(8):
                            k.mm(p, p[0:M, 0:n], Wa, Wa[:, kc, col0:col0 + M], hT[gs], hT[gs][:, kc, 0:n], kc == 0, kc == 7)
                        return p

                    for pr in range(2):
                        pa = proj(CA["RQ"] + pr * 128, 128)
                        pb = proj(CA["RQS"] + pr * 128, 128)
                        k.tt("dve", s1[pr], s1[pr][:, 0:n], pa, pa[:, 0:n], trc[gs], trc[gs][:, 0:n], ALU.mult)
                        k.tt("dve", s2[pr], s2[pr][:, 0:n], pb, pb[:, 0:n], trs[gs], trs[gs][:, 0:n], ALU.mult)
                        k.tt("pool", qk[gs], qk[gs][:, pr, 0:n], s1[pr], s1[pr][:, 0:n], s2[pr], s2[pr][:, 0:n], ALU.add)
                    for pr in range(2):
                        pa = proj(CA["RK"] + pr * 128, 128)
                        pb = proj(CA["RKS"] + pr * 128, 128)
                        k.stt(s1[pr], s1[pr][:, 0:n], pa, pa[:, 0:n], 0.125, trc[gs], trc[gs][:, 0:n], ALU.mult, ALU.mult)
                        k.stt(s2[pr], s2[pr][:, 0:n], pb, pb[:, 0:n], 0.125, trs[gs], trs[gs][:, 0:n], ALU.mult, ALU.mult)
                        k.tt("pool", qk[gs], qk[gs][:, 2 + pr, 0:n], s1[pr], s1[pr][:, 0:n], s2[pr], s2[pr][:, 0:n], ALU.add)
                    k.store("sp", RQ.g[g], RQ.ap[:, :, t0:t0 + n].rearrange("c p t -> p c t"), qk[gs], qk[gs][:, :, 0:n])
                    for c in range(4):
                        p = proj(CA["PG"] + c * 128, 128)
                        k.act(pgs[gs], pgs[gs][:, c, 0:n], p, p[:, 0:n], AF.Silu)
                    k.store("sp", PGS.g[g], PGS.ap[:, :, t0:t0 + n].rearrange("c p t -> p c t"), pgs[gs], pgs[gs][:, :, 0:n])
                    for c in range(4):
                        p = proj(CA["CB"] + c * 128, 128)
                        k.cp("act", bbs[gs], bbs[gs][:, c, 0:n], p, p[:, 0:n])
                    k.store("sp", BBs.g[g], BBs.ap[:, :, t0:t0 + n].rearrange("c p t -> p c t"), bbs[gs], bbs[gs][:, :, 0:n])
                    for c in range(4):
                        pc = proj(CA["CC"] + c * 128, 128)
                        px = proj(CA["CX"] + c * 128, 128)
                        k.cp("act", csb[c % 2], csb[c % 2][:, 0:n], pc, pc[:, 0:n])
                        k.tt("dve", uu[gs], uu[gs][:, c, 0:n], px, px[:, 0:n], csb[c % 2], csb[c % 2][:, 0:n], ALU.mult)
                    k.store("sp", UU.g[g], UU.ap[:, :, t0:t0 + n].rearrange("c p t -> p c t"), uu[gs], uu[gs][:, :, 0:n])
                    for j in range(ntile):
                        p = pp.nxt()
                        for kc in range(8):
                            k.mm(p, p[:, :], hT[gs], hT[gs][:, kc, j * 128:(j + 1) * 128], Wa, Wa[:, kc, CA["RV"]:CA["RV"] + 512], kc == 0, kc == 7)
                        k.cp("act", rvs[gs], rvs[gs][:, j, :], p, p[:, :])
                        for pr in range(2):
                            k.tr(ptk, ptk[:, pr * 128:(pr + 1) * 128], qk[gs], qk[gs][:, 2 + pr, j * 128:(j + 1) * 128], identb, identb[:], inc=(pr == 1))
                        for d_ in range(2):
                            k.tt("dve", kz[gs], kz[gs][:, j, d_, :].rearrange("p (h d) -> p h d", h=4),
                                 ptk, ptk[:, :].rearrange("p (h d) -> p h d", h=4),
                                 zt, zt[:, l, d_ * 4:(d_ + 1) * 4].unsqueeze(2).to_broadcast([128, 4, 64]), ALU.mult)
                    k.store("sp", RV.g[g], RV.ap[t0:t0 + n, :].rearrange("(j p) c -> p j c", p=128), rvs[gs], rvs[gs][:, 0:ntile, :])
                    k.store("sp", RKZ.g[g], RKZ.ap[t0:t0 + n, :, :].rearrange("(j p) d c -> p j d c", p=128), kz[gs], kz[gs][:, 0:ntile, :, :])
                k.barrier()
            if stop_after == ("P1a", l):
                break

            with contextlib.ExitStack() as pst:
                k.phase_begin(pst)
                Wb = k.sb("Wb", [128, 8, NCB], BF16)
                PW = NCB // 8
                for i in range(8):
                    k.load("pool", Wb, Wb[:, :, i * PW:(i + 1) * PW],
                           w_inb[l, :, i * PW:(i + 1) * PW].rearrange("(kc p) n -> p kc n", p=128))
                wuq = k.sb("wuq", [128, 3, 1536], BF16)
                for c in range(3):
                    k.load("pool", wuq, wuq[:, c, :], w_uqe[l, c * 128:(c + 1) * 128, :])
                wkn = k.sb("wkn", [128, 2, 512], BF16)
                wvv = k.sb("wvv", [128, 2, 512], BF16)
                k.load("pool", wkn, wkn[:], w_ukn[l, :, :].rearrange("(kc p) n -> p kc n", p=128))
                k.load("pool", wvv, wvv[:], w_ukvv[l, :, :].rearrange("(kc p) n -> p kc n", p=128))
                hT = [k.sb("hTb%d" % i, [128, 8, 512], BF16) for i in range(2)]
                c96 = k.sb("c96", [96, 512], F32); s96 = k.sb("s96", [96, 512], F32)
                c32 = k.sb("c32", [32, 512], F32); s32 = k.sb("s32", [32, 512], F32)
                sqq = k.sb("sqq", [128, 3, 512], BF16); pqn = k.sb("pqn", [128, 3, 512], BF16)
                sqkv = k.sb("sqkv", [128, 2, 512], BF16); pkvn = k.sb("pkvn", [128, 2, 512], BF16)
                rtmp = k.sb("rtmp", [128, 512], F32)
                rstdq = k.sb("rstdq", [128, 512], F32); rkvb = k.sb("rkvb", [128, 512], F32)
                rkvt = k.sb("rkvt", [128, 8], F32)
                s1 = [k.sb("b_s1_%d" % i, [128, 512], F32) for i in range(2)]
                s2 = [k.sb("b_s2_%d" % i, [128, 512], F32) for i in range(2)]
                krs = k.sb("krs", [32, 512], BF16)
                qts = k.sb("qts", [96, 8, 512], BF16)
                kns = k.sb("kns", [64, 8, 512], BF16)
                vs = k.sb("vs", [128, 4, 8, 65], BF16)
                gts = [k.sb("gts%d" % i, [128, 6, 512], BF16) for i in range(2)]
                pp = RR([k.ps("ppb%d" % i, [128, 512], F32) for i in range(7)])
                pst4 = k.ps("pst4", [128, 4], F32)
                k.memset("pool", vs, vs[:], 1.0)
                for g, (t0, n) in enumerate(groups):
                    gs = g % 2
                    ntile = n // 128
                    if g == 0:
                        k.load("sp", hT[0], hT[0][:, :, 0:n], HT.ap[:, :, t0:t0 + n].rearrange("c p t -> p c t"), src=HT.g[0])
                    if g + 1 < NG:
                        t0n, nn = groups[g + 1]
                        k.load("sp", hT[(g + 1) % 2], hT[(g + 1) % 2][:, :, 0:nn], HT.ap[:, :, t0n:t0n + nn].rearrange("c p t -> p c t"), src=HT.g[g + 1])
                    k.load("sp", c96, c96[:, 0:n], tabm[0, :, t0:t0 + n]); k.load("sp", s96, s96[:, 0:n], tabm[1, :, t0:t0 + n])
                    k.load("sp", c32, c32[:, 0:n], tabm[0, 64:96, t0:t0 + n]); k.load("sp", s32, s32[:, 0:n], tabm[1, 64:96, t0:t0 + n])

                    if CUT == 11:
                        continue

                    def proj(col0, M):
                        p = pp.nxt()
                        for kc in range(8):
                            k.mm(p, p[0:M, 0:n], Wb, Wb[:, kc, col0:col0 + M], hT[gs], hT[gs][:, kc, 0:n], kc == 0, kc == 7)
                        return p

                    for c in range(3):
                        p = proj(CBk["QD"] + c * 128, 128)
                        if CUT != 122:
                            k.act(sqq, sqq[:, c, 0:n], p, p[:, 0:n], AF.Square)
                        if CUT != 121:
                            k.ts("dve", pqn, pqn[:, c, 0:n], p, p[:, 0:n], qnt[:, l, c:c + 1], None, ALU.mult, rd=[qnt])
                    for c in range(2):
                        p = proj(CBk["KVD"] + c * 128, 128)
                        if CUT != 122:
                            k.act(sqkv, sqkv[:, c, 0:n], p, p[:, 0:n], AF.Square)
                        if CUT != 121:
                            k.ts("dve", pkvn, pkvn[:, c, 0:n], p, p[:, 0:n], kvnt[:, l, c:c + 1], None, ALU.mult, rd=[kvnt])
                    if CUT in (12, 121, 122):
                        continue
                    p = pp.nxt()
                    for c in range(3):
                        k.mm(p, p[:, 0:n], onesb, onesb[:], sqq, sqq[:, c, 0:n], c == 0, c == 2)
                    rsqrt_big(rstdq, rstdq[:, 0:n], p, p[:, 0:n], 96.0 / 384.0, 96.0 * EPS, rtmp, rtmp[:, 0:n])
                    p = pp.nxt()
                    for c in range(2):
                        k.mm(p, p[:, 0:n], onesb, onesb[:], sqkv, sqkv[:, c, 0:n], c == 0, c == 1)
                    rsqrt_big(rkvb, rkvb[:, 0:n], p, p[:, 0:n], 1.0 / 256.0, EPS, rtmp, rtmp[:, 0:n])
                    if CUT == 13:
                        continue
                    for j in range(ntile):
                        for c in range(2):
                            k.mm(pst4, pst4[:, j:j + 1], sqkv, sqkv[:, c, j * 128:(j + 1) * 128], onesb, onesb[:, 0:1], c == 0, c == 1)
                    rsqrt_chain(rkvt, rkvt[:, 4:4 + ntile], pst4, pst4[:, 0:ntile], 1.0 / 256.0, EPS, rkvt, rkvt[:, 0:ntile], mhalf[:, 0:ntile])
                    if CUT == 1:
                        continue
                    pa = proj(CBk["KR"], 32)
                    pb = proj(CBk["KRS"], 32)
                    k.tt("dve", s1[0], s1[0][0:32, 0:n], pa, pa[0:32, 0:n], c32, c32[:, 0:n], ALU.mult)
                    k.tt("dve", s2[0], s2[0][0:32, 0:n], pb, pb[0:32, 0:n], s32, s32[:, 0:n], ALU.mult)
                    k.tt("pool", krs, krs[:, 0:n], s1[0], s1[0][0:32, 0:n], s2[0], s2[0][0:32, 0:n], ALU.add)
                    k.store("sp", KR.g[g], KR.ap[:, t0:t0 + n], krs, krs[:, 0:n])
                    if CUT == 2:
                        continue
                    for h in range(8):
                        hs = h % 2
                        pa = pp.nxt()
                        for c in range(3):
                            k.mm(pa, pa[0:96, 0:n], wuq, wuq[:, c, (2 * h) * 96:(2 * h + 1) * 96], pqn, pqn[:, c, 0:n], c == 0, c == 2)
                        pb = pp.nxt()
                        for c in range(3):
                            k.mm(pb, pb[0:96, 0:n], wuq, wuq[:, c, (2 * h + 1) * 96:(2 * h + 2) * 96], pqn, pqn[:, c, 0:n], c == 0, c == 2)
                        k.tt("dve", s1[hs], s1[hs][0:96, 0:n], pa, pa[0:96, 0:n], c96, c96[:, 0:n], ALU.mult)
                        k.tt("dve", s2[hs], s2[hs][0:96, 0:n], pb, pb[0:96, 0:n], s96, s96[:, 0:n], ALU.mult)
                        k.tt("pool", s1[hs], s1[hs][0:96, 0:n], s1[hs], s1[hs][0:96, 0:n], s2[hs], s2[hs][0:96, 0:n], ALU.add)
                        k.tt("pool", qts, qts[:, h, 0:n], s1[hs], s1[hs][0:96, 0:n], rstdq, rstdq[0:96, 0:n], ALU.mult)
                        pk_ = pp.nxt()
                        for c in range(2):
                            k.mm(pk_, pk_[0:64, 0:n], wkn, wkn[:, c, h * 64:(h + 1) * 64], pkvn, pkvn[:, c, 0:n], c == 0, c == 1)
                        k.tt("dve", kns, kns[:, h, 0:n], pk_, pk_[0:64, 0:n], rkvb, rkvb[0:64, 0:n], ALU.mult)
                    k.store("sp", QT.g[g], QT.ap[:, :, t0:t0 + n].rearrange("h r t -> r h t"), qts, qts[:, :, 0:n])
                    k.store("sp", KN.g[g], KN.ap[:, :, t0:t0 + n].rearrange("h r t -> r h t"), kns, kns[:, :, 0:n])
                    if CUT == 3:
                        continue
                    for j in range(ntile):
                        p = pp.nxt()
                        for c in range(2):
                            k.mm(p, p[:, :], pkvn, pkvn[:, c, j * 128:(j + 1) * 128], wvv, wvv[:, c, :], c == 0, c == 1)
                        k.ts("dve", vs, vs[:, j, :, 0:64], p, p[:, :].rearrange("p (h e) -> p h e", h=8), rkvt[:, 4 + j:5 + j], None, ALU.mult, rd=[rkvt])
                    for j in range(ntile):
                        k.store("sp", VV.g[g], VV.ap[:, t0 + j * 128:t0 + (j + 1) * 128, :].rearrange("h p e -> p h e"), vs, vs[:, j, :, :])
                    if CUT == 4:
                        continue
                    for c6 in range(4):
                        gb = gts[c6 % 2]
                        for cc in range(6):
                            c = c6 * 6 + cc
                            p = proj(CBk["GT"] + c * 128, 128)
                            k.act(gb, gb[:, cc, 0:n], p, p[:, 0:n], AF.Sigmoid)
                        k.store("sp", GT.g[g], GT.ap[c6 * 6:(c6 + 1) * 6, :, t0:t0 + n].rearrange("c p t -> p c t"), gb, gb[:, :, 0:n])
                k.barrier()
            if stop_after == ("P1b", l):
                break

            with contextlib.ExitStack() as pst:
                k.phase_begin(pst)
                rc = k.sb("rc", [128, 4, 128], F32)
                xic = k.sb("xic", [128, 2, 128], F32)
                k.load("sp", rc, rc[:], rconst[:, :, :].rearrange("a p i -> p a i"))
                k.load("sp", xic, xic[:], xicst[:, :, :].rearrange("a p i -> p a i"))
                Mdec = k.sb("Mdec", [128, 4, 128], F32)
                etmp = k.sb("etmp", [128, 2, 128], F32)
                TAB3 = k.sb("TAB3", [128, 3, 4, 128], F32)
                gCp = k.sb("gCp", [128, 2, 2], F32)
                k.memset("pool", TAB3, TAB3[:], 0.0)
                for h in range(4):
                    pr, hh = h // 2, h % 2
                    k.act(etmp, etmp[:, 0, :], rc, rc[:, 0, :], AF.Exp, rd=[lg], scale=lg[:, l, h:h + 1])
                    k.act(etmp, etmp[:, 1, :], rc, rc[:, 1, :], AF.Exp, rd=[lg], scale=lg[:, l, 4 + h:5 + h])
                    k.tt("dve", etmp, etmp[:], etmp, etmp[:], rc, rc[:, 2:4, :], ALU.mult)
                    k.tt("dve", Mdec, Mdec[:, h, :], etmp, etmp[:, 0, :], etmp, etmp[:, 1, :], ALU.add)
                    ps_ = slice(hh * 64, (hh + 1) * 64)
                    k.memset("dve", TAB3, TAB3[ps_, 0, h, :], 1.0)
                    k.act(TAB3, TAB3[ps_, 1, h, :], xic, xic[ps_, 0, :], AF.Exp, rd=[lg], scale=lg[ps_, l, h:h + 1])
                    k.act(TAB3, TAB3[ps_, 2, h, :], xic, xic[ps_, 1, :], AF.Exp, rd=[lg], scale=lg[ps_, l, 4 + h:5 + h])
                    for d_ in range(2):
                        k.cp("dve", gCp, gCp[ps_, d_, pr:pr + 1], gC, gC[ps_, l, d_ * 4 + h:d_ * 4 + h + 1])
                wrof = k.sb("wrof", [128, 4, D], F32)
                wro = k.sb("wro", [128, 4, D], BF16)
                k.load("sp", wrof, wrof[:], w_ret_o[l, :, :].rearrange("(h p) f -> p h f", p=128))
                for h in range(4):
                    k.ts("pool", wro, wro[:, h, :], wrof, wrof[:, h, :], gnt[:, l, h:h + 1], None, ALU.mult, rd=[gnt])
                SBprev = k.sb("SBprev", [128, NT, 2, 128], BF16)
                Sf = k.sb("Sf", [128, 2, 128], F32); Sb = k.sb("Sb", [128, 2, 128], F32)
                Sfb = [k.sb("Sfb%d" % i, [128, 2, 128], BF16) for i in range(2)]
                qkg = [k.sb("qkg%d" % i, [128, 4, 512], BF16) for i in range(2)]
                kzg = [k.sb("kzg%d" % i, [128, 4, 2, 256], BF16) for i in range(2)]
                rvg = [k.sb("rvg%d" % i, [128, 4, 512], BF16) for i in range(2)]
                pgg = [k.sb("pgg%d" % i, [128, 4, 512], BF16) for i in range(2)]
                kzb = [k.sb("kzb%d" % i, [128, 4, 256], BF16) for i in range(2)]
                rvb = [k.sb("rvb%d" % i, [128, 4, 512], BF16) for i in range(2)]
                Q3 = [k.sb("Q3_%d" % i, [128, 3, 4, 128], BF16) for i in range(2)]
                ms = [k.sb("ms%d" % i, [128, 4, 128], BF16) for i in range(2)]
                ysq_ = [k.sb("ysq%d" % i, [128, 512], BF16) for i in range(2)]
                rst_ = [k.sb("rst%d" % i, [128, 512], F32) for i in range(2)]
                rtm_ = [k.sb("rtm%d" % i, [128, 512], F32) for i in range(2)]
                ytm_ = [k.sb("ytm%d" % i, [128, 512], F32) for i in range(2)]
                rTg = [k.sb("rTg%d" % i, [128, 4, 512], BF16) for i in range(2)]
                yrs = [k.sb("yrs%d" % i, [128, 8, 512], BF16) for i in range(2)]
                psc = [k.ps("psc%d" % i, [128, 512], F32) for i in range(1)] * 2
                py = [k.ps("py%d" % i, [128, 512], F32) for i in range(2)]
                pu = [k.ps("pu%d" % i, [128, 512], F32) for i in range(2)]
                pssq2 = [k.ps("pssq%d" % i, [128, 512], F32) for i in range(2)]
                pyr = k.ps("pyr", [128, 512], F32)
                k.memset("dve", Sf, Sf[:], 0.0); k.memset("dve", Sb, Sb[:], 0.0)
                k.memset("pool", Sfb[0], Sfb[0][:], 0.0)

                def s_update(S, dir_, pub):
                    for pr in range(2):
                        for hh in range(2):
                            ps_ = slice(hh * 64, (hh + 1) * 64)
                            k.stt(S, S[ps_, pr, :], S, S[ps_, pr, :], gCp[ps_, dir_, pr:pr + 1], pub,
                                  pub[ps_, pr * 256 + hh * 128:pr * 256 + (hh + 1) * 128], ALU.mult, ALU.add, rd=[gCp])

                it = 0
                for g in [0] + list(range(NG - 1, 0, -1)):
                    t0, n = groups[g]
                    ntile = n // 128
                    gs = it % 2
                    it += 1
                    k.load("sp", kzb[gs], kzb[gs][:, 0:ntile, :], RKZ.ap[t0:t0 + n, 1, :].rearrange("(j p) c -> p j c", p=128), src=RKZ.g[g])
                    k.load("sp", rvb[gs], rvb[gs][:, 0:ntile, :], RV.ap[t0:t0 + n, :].rearrange("(j p) c -> p j c", p=128), src=RV.g[g])
                    for j in range(ntile - 1, -1, -1):
                        t = t0 // 128 + j
                        k.cp("act", SBprev, SBprev[:, t, :, :], Sb, Sb[:])
                        pub = pu[t % 2]
                        for pr in range(2):
                            k.mm(pub, pub[:, pr * 256:(pr + 1) * 256], kzb[gs], kzb[gs][:, j, pr * 128:(pr + 1) * 128],
                                 rvb[gs], rvb[gs][:, j, pr * 256:(pr + 1) * 256], True, True, inc=(pr == 1))
                        s_update(Sb, 1, pub)

                def stageA(t):
                    g = gi_of_tile(t)
                    t0, n = groups[g]
                    j = t - t0 // 128
                    gs = g % 2
                    if j == 0:
                        ntile = n // 128
                        k.load("sp", qkg[gs], qkg[gs][:, :, 0:n], RQ.ap[:, :, t0:t0 + n].rearrange("c p t -> p c t"), src=RQ.g[g])
                        k.load("sp", kzg[gs], kzg[gs][:, 0:ntile, :, :], RKZ.ap[t0:t0 + n, :, :].rearrange("(j p) d c -> p j d c", p=128), src=RKZ.g[g])
                        k.load("sp", rvg[gs], rvg[gs][:, 0:ntile, :], RV.ap[t0:t0 + n, :].rearrange("(j p) c -> p j c", p=128), src=RV.g[g])
                        k.load("sp", pgg[gs], pgg[gs][:, :, 0:n], PGS.ap[:, :, t0:t0 + n].rearrange("c p t -> p c t"), src=PGS.g[g])
                    cs = slice(j * 128, (j + 1) * 128)
                    q3 = Q3[t % 2]
                    for pr in range(2):
                        k.tt("dve", q3, q3[:, :, 2 * pr:2 * pr + 2, :], qkg[gs],
                             qkg[gs][:, pr, cs].unsqueeze(1).unsqueeze(1).to_broadcast([128, 3, 2, 128]),
                             TAB3, TAB3[:, :, 2 * pr:2 * pr + 2, :], ALU.mult)
                    pscb = psc[t % 2]
                    for h in range(4):
                        k.mm(pscb, pscb[:, h * 128:(h + 1) * 128], qkg[gs], qkg[gs][:, 2 + h // 2, cs], q3, q3[:, 0, h, :], True, True, inc=(h == 3))
                    k.tt("dve", ms[t % 2], ms[t % 2][:], pscb, pscb[:, :].rearrange("p (h i) -> p h i", h=4), Mdec, Mdec[:], ALU.mult)

                def stageB(t):
                    g = gi_of_tile(t)
                    t0, n = groups[g]
                    j = t - t0 // 128
                    gs = g % 2
                    cs = slice(j * 128, (j + 1) * 128)
                    q3 = Q3[t % 2]
                    pyb = py[t % 2]
                    sfb = Sfb[t % 2]
                    for h in range(4):
                        pr = h // 2
                        o = pyb[:, h * 128:(h + 1) * 128]
                        k.mm(pyb, o, rvg[gs], rvg[gs][:, j, h * 128:(h + 1) * 128], ms[t % 2], ms[t % 2][:, h, :], True, False)
                        k.mm(pyb, o, sfb, sfb[:, pr, :], q3, q3[:, 1, h, :], False, False)
                        k.mm(pyb, o, SBprev, SBprev[:, t, pr, :], q3, q3[:, 2, h, :], False, True, inc=(h == 3))
                    pub = pu[t % 2]
                    for pr in range(2):
                        k.mm(pub, pub[:, pr * 256:(pr + 1) * 256], kzg[gs], kzg[gs][:, j, 0, pr * 128:(pr + 1) * 128],
                             rvg[gs], rvg[gs][:, j, pr * 256:(pr + 1) * 256], True, True, inc=(pr == 1))
                    s_update(Sf, 0, pub)
                    k.cp("act", Sfb[(t + 1) % 2], Sfb[(t + 1) % 2][:], Sf, Sf[:])
                    ysq = ysq_[t % 2]
                    k.act(ysq, ysq[:], pyb, pyb[:], AF.Square)
                    k.mm(pssq2[t % 2], pssq2[t % 2][:], onesb, onesb[:], ysq, ysq[:], True, True)

                def stageC(t):
                    g = gi_of_tile(t)
                    t0, n = groups[g]
                    j = t - t0 // 128
                    gs = g % 2
                    cs = slice(j * 128, (j + 1) * 128)
                    pyb = py[t % 2]
                    rst = rst_[t % 2]; rtm = rtm_[t % 2]; ytm = ytm_[t % 2]; pssq = pssq2[t % 2]
                    rsqrt_big(rst, rst[:], pssq, pssq[:], 1.0 / 128.0, EPS, rtm, rtm[:])
                    k.tt("dve", ytm, ytm[:], pyb, pyb[:], rst, rst[:], ALU.mult)
                    k.tt("pool", rTg[gs], rTg[gs][:, :, cs], ytm, ytm[:].rearrange("p (h i) -> p h i", h=4), pgg[gs], pgg[gs][:, :, cs], ALU.mult)
                    if j == n // 128 - 1:
                        for fc in range(8):
                            for h in range(4):
                                k.mm(pyr, pyr[:, 0:n], wro, wro[:, h, fc * 128:(fc + 1) * 128], rTg[gs], rTg[gs][:, h, 0:n], h == 0, h == 3)
                            k.cp("act", yrs[gs], yrs[gs][:, fc, 0:n], pyr, pyr[:, 0:n])
                        k.store("sp", YR.g[g], YR.ap[:, :, t0:t0 + n].rearrange("c p t -> p c t"), yrs[gs], yrs[gs][:, :, 0:n])

                stageA(0)
                for idx in range(1, NT + 2):
                    if idx - 1 < NT:
                        stageB(idx - 1)
                    if idx < NT:
                        stageA(idx)
                    if idx >= 2:
                        stageC(idx - 2)
                k.barrier()
            if stop_after == ("P3", l):
                break

            with contextlib.ExitStack() as pst:
                k.phase_begin(pst)
                wco = k.sb("wco", [128, 4, D], BF16)
                k.load("pool", wco, wco[:], w_conv_o[l, :, :].rearrange("(c p) f -> p c f", p=128))
                ug = [k.sb("ug%d" % i, [128, 4, 514], BF16) for i in range(2)]
                bg = [k.sb("bg%d" % i, [128, 4, 512], BF16) for i in range(2)]
                cv = [k.sb("cv%d" % i, [128, 512], F32) for i in range(2)]
                cT = [k.sb("cT%d" % i, [128, 4, 512], BF16) for i in range(2)]
                ycs = [k.sb("ycs%d" % i, [128, 8, 512], BF16) for i in range(2)]
                pc_ = RR([k.ps("pcv%d" % i, [128, 512], F32) for i in range(4)])
                def load4(g):
                    t0, n = groups[g]
                    gs = g % 2
                    first = (g == 0) or (g == 1)
                    last = (g == 0) or (g == NG - 1)
                    lo = t0 if first else t0 - 1
                    hi = t0 + n if last else t0 + n + 1
                    if first:
                        k.memset("pool", ug[gs], ug[gs][:, :, 0:1], 0.0)
                    if last:
                        k.memset("pool", ug[gs], ug[gs][:, :, n + 1:n + 2], 0.0)
                    srcs = [UU.g[g]] + ([] if first else [UU.g[g - 1]]) + ([] if last else [UU.g[g + 1]])
                    k.dma("sp", ug[gs][:, :, (lo - t0 + 1):(hi - t0 + 1)], UU.ap[:, :, lo:hi].rearrange("c p t -> p c t"),
                          reads=srcs, writes=[ug[gs]], chan=k.bchan(ug[gs], "sp"))
                    k.load("sp", bg[gs], bg[gs][:, :, 0:n], BBs.ap[:, :, t0:t0 + n].rearrange("c p t -> p c t"), src=BBs.g[g])

                load4(0)
                for g, (t0, n) in enumerate(groups):
                    gs = g % 2
                    if g + 1 < NG:
                        load4(g + 1)
                    for c in range(4):
                        e = "dve" if c % 2 == 0 else "dve"
                        cvb = cv[c % 2]
                        k.ts(e, cvb, cvb[:, 0:n], ug[gs], ug[gs][:, c, 0:n], cwt[:, l, c, 0:1], None, ALU.mult, rd=[cwt])
                        k.stt(cvb, cvb[:, 0:n], ug[gs], ug[gs][:, c, 1:n + 1], cwt[:, l, c, 1:2], cvb, cvb[:, 0:n], ALU.mult, ALU.add, rd=[cwt])
                        k.stt(cvb, cvb[:, 0:n], ug[gs], ug[gs][:, c, 2:n + 2], cwt[:, l, c, 2:3], cvb, cvb[:, 0:n], ALU.mult, ALU.add, rd=[cwt])
                        k.tt("pool", cT[gs], cT[gs][:, c, 0:n], cvb, cvb[:, 0:n], bg[gs], bg[gs][:, c, 0:n], ALU.mult)
                    for fc in range(8):
                        p = pc_.nxt()
                        for c in range(4):
                            k.mm(p, p[:, 0:n], wco, wco[:, c, fc * 128:(fc + 1) * 128], cT[gs], cT[gs][:, c, 0:n], c == 0, c == 3)
                        k.cp("act", ycs[gs], ycs[gs][:, fc, 0:n], p, p[:, 0:n])
                    k.store("sp", YC.g[g], YC.ap[:, :, t0:t0 + n].rearrange("c p t -> p c t"), ycs[gs], ycs[gs][:, :, 0:n])
                k.barrier()
            if stop_after == ("P4", l):
                break

            with contextlib.ExitStack() as pst:
                k.phase_begin(pst)
                KTh = [k.sb("KTh%d" % i, [96, T], BF16) for i in range(2)]
                QTh = [k.sb("QTh%d" % i, [96, T], BF16) for i in range(2)]
                Vh = [k.sb("Vh%d" % i, [128, NT, 128], BF16) for i in range(2)]
                for i_ in range(2):
                    k.memset("pool", Vh[i_], Vh[i_][:], 0.0)
                pT = [k.sb("pT%d" % i, [128, 512], BF16) for i in range(4)]
                osb = [k.sb("osb%d" % i, [65, 512], F32) for i in range(2)]
                rden = k.sb("rden", [64, 512], F32)
                onb = [k.sb("onb%d" % i, [64, 512], BF16) for i in range(2)]
                sel = k.sb("sel", [65, 64], F32)
                k.memset("pool", sel, sel[:], 0.0)
                k.memset("pool", sel, sel[64:65, :], 1.0)
                pss = RR([k.ps("pss%d" % i, [128, 512], F32) for i in range(5)])
                pso = [k.ps("pso%d" % i, [128, 512], F32) for i in range(2)]
                pden = k.ps("pden", [128, 512], F32)
                allg = list(range(NG))
                qgroups = list(range(NG)) if l == 0 else list(range(1, NG))
                SK = 2
                qi = 0
                def load_head(h):
                    hs = h % 2
                    k.dma("sp", KTh[hs][0:64, :], KN.ap[h, :, :], reads=[KN.g[g_] for g_ in allg], writes=[KTh[hs]], chan=_ch(k, KTh[hs]))
                    k.dma("sp", KTh[hs][64:96, :], KR.ap[:, :], reads=[KR.g[g_] for g_ in allg], writes=[KTh[hs]], chan=_ch(k, KTh[hs]))
                    k.dma("sp", QTh[hs][:, :], QT.ap[h, :, :], reads=[QT.g[g_] for g_ in allg], writes=[QTh[hs]], chan=_ch(k, QTh[hs]))
                    k.dma("sp", Vh[hs][:, :, 0:65], VV.ap[h, :, :].rearrange("(t p) e -> p t e", p=128), reads=[VV.g[g_] for g_ in allg],
                          writes=[Vh[hs]], chan=_ch(k, Vh[hs]))

                load_head(0)
                for h in range(8):
                    hs = h % 2
                    pr, hh = h // 2, h % 2
                    if h + 1 < 8:
                        load_head(h + 1)
                    for g in qgroups:
                        q0, nq = groups[g]
                        nkt = 2 if g == 0 else NT
                        po = pso[qi % 2]
                        pSs = {}
                        for kt in range(nkt + SK):
                            if kt < nkt:
                                pS = pss.nxt()
                                k.mm(pS, pS[:, 0:nq], KTh[hs], KTh[hs][:, kt * 128:(kt + 1) * 128], QTh[hs], QTh[hs][:, q0:q0 + nq], True, True)
                                k.act(pT[kt % 4], pT[kt % 4][:, 0:nq], pS, pS[:, 0:nq], AF.Exp)
                            if kt >= SK:
                                kk = kt - SK
                                k.mm(po, po[:, 0:nq], Vh[hs], Vh[hs][:, kk, :], pT[kk % 4], pT[kk % 4][:, 0:nq], kk == 0, kk == nkt - 1)
                        ob = osb[qi % 2]
                        k.cp("dve", ob, ob[:, 0:nq], po, po[0:65, 0:nq])
                        k.mm(pden, pden[0:64, 0:nq], sel, sel[:, :], ob, ob[:, 0:nq], True, True)
                        k.op("dve", lambda: nc.vector.reciprocal(out=rden[:, 0:nq], in_=pden[0:64, 0:nq]), reads=[pden], writes=[rden])
                        k.tt("pool", onb[qi % 2], onb[qi % 2][:, 0:nq], ob, ob[0:64, 0:nq], rden, rden[:, 0:nq], ALU.mult)
                        k.store("sp", OT.g[g], OT.ap[pr, hh * 64:(hh + 1) * 64, q0:q0 + nq], onb[qi % 2], onb[qi % 2][:, 0:nq])
                        qi += 1
                k.barrier()
            if stop_after == ("P5", l):
                break

            with contextlib.ExitStack() as pst:
                k.phase_begin(pst)
                wmo = k.sb("wmo", [128, 4, D], BF16)
                k.load("pool", wmo, wmo[:], w_mla_o[l, :, :].rearrange("(c p) f -> p c f", p=128))
                wout = k.sb("wout", [128, 8, D], BF16)
                k.load("pool", wout, wout[:], w_out[l, :, :].rearrange("(c p) f -> p c f", p=128))
                wr = k.sb("wr", [128, 8, NE], F32)
                k.load("sp", wr, wr[:], w_router[:, :].rearrange("(c p) e -> p c e", p=128))
                g1b = [k.sb("g1b%d" % i, [128, D], F32) for i in range(2)]
                for r in range(2):
                    k.load("sp", g1b[r], g1b[r][:], MOD[l, r:r + 1, 2 * D:3 * D].partition_broadcast(128), src=MOD)
                otg = [k.sb("otg%d" % i, [128, 4, 256], BF16) for i in range(2)]
                yrg = [k.sb("yrg%d" % i, [128, 8, 256], BF16) for i in range(2)]
                ycg = [k.sb("ycg%d" % i, [128, 8, 256], BF16) for i in range(2)]
                gtg2 = [[k.sb("gtg%d_%d" % (i, b_), [128, 8, 256], BF16) for b_ in range(3)] for i in range(2)]
                a1 = [k.sb("a1_%d" % i, [128, 512], F32) for i in range(2)]
                a2 = [k.sb("a2_%d" % i, [128, 512], F32) for i in range(2)]
                a3 = [k.sb("a3_%d" % i, [128, 512], F32) for i in range(2)]
                mT = k.sb("mT", [128, 8, 512], BF16)
                xin = [k.sb("xin6_%d" % i, [128, D], F32) for i in range(2)]
                xm = [k.sb("xm%d" % i, [128, D], F32) for i in range(2)]
                tt1_ = [k.sb("tt1_%d" % i, [128, D], F32) for i in range(2)]
                junk = k.sb("junk6", [128, D], BF16)
                st6 = [k.sb("st6_%d" % i, [128, 8], F32) for i in range(2)]
                xn2_ = [k.sb("xn2_%d" % i, [128, D], F32) for i in range(2)]
                h2f_ = [k.sb("h2f_%d" % i, [128, 8, 128], F32) for i in range(2)]
                h2m_ = [k.sb("h2m_%d" % i, [128, 8, 128], F32) for i in range(1)] * 2
                g2nb = [k.sb("g2nb%d" % i, [128, D], F32) for i in range(2)]
                sh2b = [k.sb("sh2b%d" % i, [128, D], F32) for i in range(2)]
                n2b = k.sb("n2b", [128, D], F32)
                k.load("sp", n2b, n2b[:], norm2_r[l, 0:1, :].partition_broadcast(128))
                for r in range(2):
                    k.load("sp", g2nb[r], g2nb[r][:], MOD[l, r:r + 1, 4 * D:5 * D].partition_broadcast(128), src=MOD)
                    k.load("sp", sh2b[r], sh2b[r][:], MOD[l, r:r + 1, 3 * D:4 * D].partition_broadcast(128), src=MOD)
                    k.stt(g2nb[r], g2nb[r][:], g2nb[r], g2nb[r][:], 1.0, n2b, n2b[:], ALU.add, ALU.mult)
                h2t_ = tt1_
                h2tb_ = [k.sb("h2tb%d" % i, [128, D], BF16) for i in range(2)]
                gslb_ = [k.sb("gslb%d" % i, [128, 4], BF16) for i in range(2)]
                k.memset("dve", carry, carry[:], 0.0)
                rt_ = [k.sb("rt%d" % i, [128, 16, 16], F32) for i in range(2)]
                gto = [k.sb("gto%d" % i, [128, NE], F32) for i in range(2)]
                pm = RR([k.ps("pm6_%d" % i, [128, 512], F32) for i in range(3)])
                pox_ = [k.ps("pox%d" % i, [128, D], F32) for i in range(1)] * 2
                ptf = k.ps("ptf", [128, 8, 128], F32)
                plg_ = k.ps("plg", [128, 2, 32], F32)
                ti = 0
                units6 = []
                for g in (range(NG) if l == 0 else range(1, NG)):
                    t0, n = groups[g]
                    if n == 512:
                        units6 += [(g, t0, 256), (g, t0 + 256, 256)]
                    else:
                        units6.append((g, t0, n))

                def load6(u):
                    g, t0, n = units6[u]
                    us = u % 2
                    k.load("sp", otg[us], otg[us][:, :, 0:n], OT.ap[:, :, t0:t0 + n].rearrange("c p t -> p c t"), src=OT.g[g])
                    k.load("sp", yrg[us], yrg[us][:, :, 0:n], YR.ap[:, :, t0:t0 + n].rearrange("c p t -> p c t"), src=YR.g[g])
                    k.load("sp", ycg[us], ycg[us][:, :, 0:n], YC.ap[:, :, t0:t0 + n].rearrange("c p t -> p c t"), src=YC.g[g])
                    for b_ in range(3):
                        k.load("sp", gtg2[us][b_], gtg2[us][b_][:, :, 0:n], GT.ap[b_ * 8:(b_ + 1) * 8, :, t0:t0 + n].rearrange("c p t -> p c t"), src=GT.g[g])

                load6(0)
                for u, (g, t0, n) in enumerate(units6):
                    gs = u % 2
                    gtg = gtg2[gs]
                    r = 1 if g == 0 else 0
                    ntile = n // 128
                    if u + 1 < len(units6):
                        load6(u + 1)
                    for fc in range(8):
                        fs = fc % 2
                        p = pm.nxt()
                        for pr in range(4):
                            k.mm(p, p[:, 0:n], wmo, wmo[:, pr, fc * 128:(fc + 1) * 128], otg[gs], otg[gs][:, pr, 0:n], pr == 0, pr == 3)
                        k.tt("dve", a3[fs], a3[fs][:, 0:n], p, p[:, 0:n], gtg[2], gtg[2][:, fc, 0:n], ALU.mult)
                        k.tt("dve", a1[fs], a1[fs][:, 0:n], yrg[gs], yrg[gs][:, fc, 0:n], gtg[0], gtg[0][:, fc, 0:n], ALU.mult)
                        k.tt("pool", a2[fs], a2[fs][:, 0:n], ycg[gs], ycg[gs][:, fc, 0:n], gtg[1], gtg[1][:, fc, 0:n], ALU.mult)
                        k.tt("pool", a1[fs], a1[fs][:, 0:n], a1[fs], a1[fs][:, 0:n], a2[fs], a2[fs][:, 0:n], ALU.add)
                        k.tt("dve", mT, mT[:, fc, 0:n], a1[fs], a1[fs][:, 0:n], a3[fs], a3[fs][:, 0:n], ALU.add)
                    tbase = ti
                    ti += ntile

                    def t_out(j):
                        xs = (tbase + j) % 2
                        cs = slice(j * 128, (j + 1) * 128)
                        pox = pox_[xs]
                        for half in range(2):
                            for kc in range(8):
                                k.mm(pox, pox[:, half * 512:(half + 1) * 512], mT, mT[:, kc, cs], wout, wout[:, kc, half * 512:(half + 1) * 512], kc == 0, kc == 7)

                    def t_mid(j):
                        t = t0 // 128 + j
                        xs = (tbase + j) % 2
                        tt1 = tt1_[xs]; xn2 = xn2_[xs]; pox = pox_[xs]; st = st6[xs]
                        sap, sbuf_ = res_src(l, t)
                        k.load("sp", xin[xs], xin[xs][:], sap, src=sbuf_)
                        k.tt("dve", tt1, tt1[:], pox, pox[:], g1b[r], g1b[r][:], ALU.mult)
                        k.tt("dve", xm[xs], xm[xs][:], tt1, tt1[:], xin[xs], xin[xs][:], ALU.add)
                        k.store("sp", XM.g[g], XM.ap[t * 128:(t + 1) * 128, :], xm[xs], xm[xs][:])
                        k.act(junk, junk[:], xm[xs], xm[xs][:], AF.Square, accb=st, accum_out=st[:, 0:1])
                        k.act(st, st[:, 1:2], st, st[:, 0:1], AF.Ln, scale=1.0 / D, bias=EPS)
                        k.act(st, st[:, 2:3], st, st[:, 1:2], AF.Exp, scale=-0.5)
                        k.ts("dve", xn2, xn2[:], xm[xs], xm[xs][:], st[:, 2:3], None, ALU.mult, rd=[st])

                    def t_tr(j):
                        t = t0 // 128 + j
                        xs = (tbase + j) % 2
                        xn2 = xn2_[xs]; h2f = h2f_[xs]
                        for c in range(8):
                            k.tr(ptf, ptf[:, c, :], xn2, xn2[:, c * 128:(c + 1) * 128], ident32, ident32[:], inc=(c == 7))
                        for c in range(8):
                            k.act(h2f, h2f[:, c, :], ptf, ptf[:, c, :], AF.Identity, rd=[vecs],
                                  scale=vecs[:, l, r, 2, c:c + 1], bias=vecs[:, l, r, 3, c:c + 1])
                        h2t = h2t_[xs]; h2tb = h2tb_[xs]
                        k.tt("dve", h2t, h2t[:], xn2, xn2[:], g2nb[r], g2nb[r][:], ALU.mult)
                        k.tt("pool", h2tb, h2tb[:], h2t, h2t[:], sh2b[r], sh2b[r][:], ALU.add)
                        k.store("sp", H2R.g[g], H2R.ap[t * 128:(t + 1) * 128, :], h2tb, h2tb[:])

                    def t_rt_ops(j):
                        ops = []
                        tail = []
                        t = t0 // 128 + j
                        xs = (tbase + j) % 2
                        h2f = h2f_[xs]; rt = rt_[xs]; gslb = gslb_[xs]
                        plg = plg_
                        for kc in range(8):
                            k.mm(plg, plg[:, xs, 0:16], h2f, h2f[:, kc, :], wr, wr[:, kc, :], kc == 0, kc == 7)
                        sc_ = rt[:, 0, :]; bs = rt[:, 1, :]; eq1 = rt[:, 2, :]; bs2 = rt[:, 3, :]; eq2 = rt[:, 4, :]
                        m1 = rt[:, 5, 0:4]; m2 = rt[:, 5, 4:8]; gsm = rt[:, 5, 8:12]; gmx = rt[:, 5, 12:13]; wsm = rt[:, 5, 13:14]; rws = rt[:, 5, 14:15]
                        gsl = rt[:, 6, 0:4]; msk = rt[:, 7, :]; wgt = rt[:, 8, :]; ex = rt[:, 10, :]
                        v3 = lambda ap: ap.rearrange("p (g e) -> p g e", g=4)
                        bc4 = lambda ap: ap.unsqueeze(2).to_broadcast([128, 4, 4])
                        ops.append(lambda: k.act(rt, ex, plg, plg[:, xs, 0:16], AF.Exp, scale=-1.0))
                        ops.append(lambda: k.ts("dve", rt, ex, rt, ex, 1.0, None, ALU.add))
                        ops.append(lambda: k.op("dve", lambda: nc.vector.reciprocal(out=sc_, in_=ex), reads=[rt], writes=[rt]))
                        ops.append(lambda: k.tt("dve", rt, bs, rt, sc_, rbias, rbias[:], ALU.add))
                        ops.append(lambda: k.op("dve", lambda: nc.vector.tensor_reduce(out=m1, in_=v3(bs), axis=AX.X, op=ALU.max), reads=[rt], writes=[rt]))
                        ops.append(lambda: k.tt("dve", rt, v3(eq1), rt, v3(bs), rt, bc4(m1), ALU.is_equal))
                        ops.append(lambda: k.stt(rt, bs2, rt, eq1, -1.0e9, rt, bs, ALU.mult, ALU.add))
                        ops.append(lambda: k.op("dve", lambda: nc.vector.tensor_reduce(out=m2, in_=v3(bs2), axis=AX.X, op=ALU.max), reads=[rt], writes=[rt]))
                        ops.append(lambda: k.tt("dve", rt, gsm, rt, m1, rt, m2, ALU.add))
                        ops.append(lambda: k.op("dve", lambda: nc.vector.tensor_reduce(out=gmx, in_=gsm, axis=AX.X, op=ALU.max), reads=[rt], writes=[rt]))
                        ops.append(lambda: k.ts("dve", rt, gsl, rt, gsm, gmx, None, ALU.is_equal))
                        ops.append(lambda: k.tt("dve", rt, v3(eq2), rt, v3(bs2), rt, bc4(m2), ALU.is_equal))
                        ops.append(lambda: k.tt("dve", rt, msk, rt, eq1, rt, eq2, ALU.add))
                        ops.append(lambda: k.tt("dve", rt, v3(msk), rt, v3(msk), rt, bc4(gsl), ALU.mult))
                        ops.append(lambda: k.tt("dve", rt, wgt, rt, sc_, rt, msk, ALU.mult))
                        ops.append(lambda: k.op("dve", lambda: nc.vector.tensor_reduce(out=wsm, in_=wgt, axis=AX.X, op=ALU.add), reads=[rt], writes=[rt]))
                        ops.append(lambda: k.op("dve", lambda: nc.vector.reciprocal(out=rws, in_=wsm), reads=[rt], writes=[rt]))
                        ops.append(lambda: k.ts("dve", gto[xs], gto[xs][:], rt, wgt, rws, None, ALU.mult, rd=[rt]))
                        ops.append(lambda: k.op("dve", lambda: nc.vector.tensor_reduce(out=G16A[:, t, 0:4], in_=gto[xs][:].rearrange("p (g e) -> p e g", g=4), axis=AX.X, op=ALU.add),
                             reads=[gto[xs]], writes=[G16A]))
                        ops.append(lambda: k.cp("dve", GSLA, GSLA[:, t, :], rt, gsl))
                        ops.append(lambda: k.cp("dve", gslb, gslb[:], rt, gsl))
                        tail.append(lambda: k.mm(plg, plg[:, xs, 16:20], ustrb, ustrb[:], gslb, gslb[:], True, True, inc=False))
                        tail.append(lambda: k.mm(plg, plg[:, xs, 20:24], onesb, onesb[:], gslb, gslb[:], True, True))
                        rkf = rt[:, 9, 0:4]; rkm = rt[:, 9, 4:8]
                        tail.append(lambda: k.tt("dve", rt, rkf, plg, plg[:, xs, 16:20], carry, carry[:], ALU.add))
                        tail.append(lambda: k.tt("dve", rt, rkm, rt, rkf, rt, gsl, ALU.mult))
                        tail.append(lambda: k.op("dve", lambda: nc.vector.tensor_reduce(out=RANKA[:, t:t + 1], in_=rkm, axis=AX.X, op=ALU.add), reads=[rt], writes=[RANKA]))
                        tail.append(lambda: k.tt("dve", carry, carry[:], carry, carry[:], plg, plg[:, xs, 20:24], ALU.add))
                        return ops, tail

                    seq = []
                    for j in range(ntile + 3):
                        if j < ntile:
                            seq.append((t_out, j))
                        if 1 <= j <= ntile:
                            seq.append((t_tr, j - 1))
                        if j < ntile:
                            seq.append((t_mid, j))
                        if 2 <= j <= ntile + 1:
                            seq.append(("rt", j - 2))
                    pend = []
                    for fn_, j_ in seq:
                        if fn_ == "rt":
                            pend.append(t_rt_ops(j_))
                            if len(pend) == 2 or j_ == ntile - 1:
                                n_ops = max(len(p_[0]) for p_ in pend)
                                for oi_ in range(n_ops):
                                    for p_ in pend:
                                        if oi_ < len(p_[0]):
                                            p_[0][oi_]()
                                for p_ in pend:
                                    for f_ in p_[1]:
                                        f_()
                                pend = []
                        else:
                            fn_(j_)
                k.barrier()
            if stop_after == ("P6", l):
                break

            tiles_l = list(range(NT)) if l == 0 else list(range(2, NT))
            with contextlib.ExitStack() as pst:
                k.phase_begin(pst)
                t49 = k.sb("t49", [128, 4, 9], F32)
                nbv = k.sb("nbv", [128, 4], F32)
                base = k.sb("base", [128, 4], F32)
                gbf = k.sb("gbf", [128, NB], F32)
                gbt = k.sb("gbt", [128, NB], F32)
                idxf = k.sb("idxf", [128, NB, 4], F32)
                slt = k.sb("slt", [128, NT, 4], F32)
                slf = k.sb("slf", [128, NT], F32)
                zt16 = k.sb("zt16", [128, 8 * D], BF16)
                zg = k.sb("zg", [128, (NS // 128) * 16], F32)
                hb2 = [k.sb("hb2_%d" % i, [128, D], BF16) for i in range(6)]
                k.tt("dve", t49, t49[:], carry, carry[:, :].unsqueeze(2).to_broadcast([128, 4, 9]),
                     mc, mc[:, 0:9].unsqueeze(1).to_broadcast([128, 4, 9]), ALU.is_gt)
                k.op("dve", lambda: nc.vector.tensor_reduce(out=nbv[:], in_=t49[:], axis=AX.X, op=ALU.add), reads=[t49], writes=[nbv])
                k.memset("dve", base, base[:, 0:1], 0.0)
                for g_ in range(1, 4):
                    k.stt(base, base[:, g_:g_ + 1], nbv, nbv[:, g_ - 1:g_], 1024.0, base, base[:, g_ - 1:g_], ALU.mult, ALU.add)
                k.memset("dve", gbf, gbf[:], 0.0)
                for g_ in range(1, 4):
                    k.ts("dve", gbt, gbt[:], mc, mc[:, 16:16 + NB], base[:, g_:g_ + 1], None, ALU.is_ge, rd=[base])
                    k.tt("dve", gbf, gbf[:], gbf, gbf[:], gbt, gbt[:], ALU.add)
                k.stt(idxf, idxf[:], gbf, gbf[:, :].unsqueeze(2).to_broadcast([128, NB, 4]), 512.0,
                      mc, mc[:, 40:44].unsqueeze(1).to_broadcast([128, NB, 4]), ALU.mult, ALU.add)
                if l > 0:
                    k.ts("dve", idxf, idxf[:], idxf, idxf[:], float(l * NE * 128), None, ALU.add)
                k.cp("dve", IDXW, IDXW[:], idxf, idxf[:])
                k.tt("dve", slt, slt[:], GSLA, GSLA[:], base, base[:, :].unsqueeze(1).to_broadcast([128, NT, 4]), ALU.mult)
                k.op("dve", lambda: nc.vector.tensor_reduce(out=slf[:], in_=slt[:], axis=AX.X, op=ALU.add), reads=[slt], writes=[slf])
                k.tt("dve", slf, slf[:], slf, slf[:], RANKA, RANKA[:], ALU.add)
                k.cp("dve", SLOTI, SLOTI[:], slf, slf[:])
                k.memset("pool", zt16, zt16[:], 0.0)
                k.memset("pool", zg, zg[:], 0.0)
                zfill = Buf("zfill")
                for b_ in range(NB):
                    k.dma("sp", H2S[b_ * 1024:(b_ + 1) * 1024, :].rearrange("(p j) f -> p (j f)", p=128), zt16[:], reads=[zt16], writes=[zfill],
                          chan=k.bchan(zt16, "sp"))
                k.dma("sp", G4S[:, :].rearrange("(p j) e -> p (j e)", p=128), zg[:], reads=[zg], writes=[zfill], chan=k.bchan(zg, "sp"))
                for i_, t in enumerate(tiles_l):
                    hb = hb2[i_ % 6]
                    k.load("sp", hb, hb[:], H2R.ap[t * 128:(t + 1) * 128, :], src=H2R.g[gi_of_tile(t)])
                    k.dma("pool", H2S[:, :], hb[:], reads=[hb, SLOTI, zfill], writes=[], chan=k.bchan(hb, "pool"), idx=("scatter", SLOTI[:, t:t + 1]))
                    k.dma("pool", G4S[:, :], G16A[:, t, :], reads=[G16A, SLOTI, zfill], writes=[], chan=k.bchan(G16A, "pool"), idx=("scatter", SLOTI[:, t:t + 1]))
                k.barrier()
            if stop_after == ("P6b", l):
                break

            with contextlib.ExitStack() as pst:
                k.phase_begin(pst)
                w1e = [k.sb("w1e%d" % i, [128, 8, 512], BF16) for i in range(2)]
                w3e = [k.sb("w3e%d" % i, [128, 8, 512], BF16) for i in range(2)]
                w2e = [k.sb("w2e%d" % i, [128, 4, D], BF16) for i in range(2)]
                hblk = [k.sb("hblk%d" % i, [128, 8, D], BF16) for i in range(2)]
                g4b = [k.sb("g4b%d" % i, [128, 8, 16], F32) for i in range(2)]
                h2T = [k.sb("h2T%d" % i, [128, 8, 1024], BF16) for i in range(2)]
                acc = k.sb("acc", [128, 8, D], F32)
                sl = [k.sb("sl%d" % i, [128, 512], F32) for i in range(2)]
                zT = [k.sb("zT%d" % i, [128, 4, 512], BF16) for i in range(2)]
                pab = RR([k.ps("pab%d" % i, [128, 512], F32) for i in range(4)])
                ph = RR([k.ps("ph%d" % i, [128, 512], F32) for i in range(3)])
                ptr = k.ps("ptr7", [128, 8, 128], BF16)

                def load_w(b_, ee, which):
                    es = (b_ * 4 + ee) % 2
                    ix = IDXW[:, b_, ee:ee + 1]
                    if which == 0:
                        k.dma("pool", w1e[es][:].rearrange("p a b -> p (a b)"), w1r[:, :, :].rearrange("l r c -> (l r) c"), reads=[IDXW], writes=[w1e[es]],
                              chan=k.bchan(w1e[es], "pool"), idx=("gather", ix))
                        k.dma("pool", w3e[es][:].rearrange("p a b -> p (a b)"), w3r[:, :, :].rearrange("l r c -> (l r) c"), reads=[IDXW], writes=[w3e[es]],
                              chan=k.bchan(w3e[es], "pool"), idx=("gather", ix))
                    else:
                        k.dma("pool", w2e[es][:].rearrange("p a b -> p (a b)"), w2r[:, :, :].rearrange("l r c -> (l r) c"), reads=[IDXW], writes=[w2e[es]],
                              chan=k.bchan(w2e[es], "pool"), idx=("gather", ix))

                def load_blk(b_):
                    bs = b_ % 2
                    k.load("sp", hblk[bs], hblk[bs][:], H2S[b_ * 1024:(b_ + 1) * 1024, :].rearrange("(j p) f -> p j f", p=128))
                    k.load("sp", g4b[bs], g4b[bs][:], G4S[b_ * 1024:(b_ + 1) * 1024, :].rearrange("(j p) e -> p j e", p=128))

                def transp_blk(b_):
                    bs = b_ % 2
                    for j in range(8):
                        for c in range(8):
                            k.tr(ptr, ptr[:, c, :], hblk[bs], hblk[bs][:, j, c * 128:(c + 1) * 128], identb, identb[:], inc=(c == 7))
                        k.cp("act" if j % 2 == 0 else "dve", h2T[bs], h2T[bs][:, :, j * 128:(j + 1) * 128], ptr, ptr[:])

                units = [(b_, ee, half) for b_ in range(NB) for ee in range(4) for half in range(2)]

                def stage_a(ui):
                    b_, ee, half = units[ui]
                    bs = b_ % 2
                    es = (b_ * 4 + ee) % 2
                    zs = ui % 2
                    c0 = half * 512
                    if half == 0:
                        if ee == 0 and b_ == 0:
                            load_blk(0)
                            load_w(0, 0, 0)
                            load_w(0, 0, 1)
                            transp_blk(0)
                        nb_, ne_ = (b_, ee + 1) if ee < 3 else (b_ + 1, 0)
                        if nb_ < NB:
                            load_w(nb_, ne_, 0)
                        if ee == 1 and b_ + 1 < NB:
                            load_blk(b_ + 1)
                    for jc in range(4):
                        pa = pab.nxt()
                        for kc in range(8):
                            k.mm(pa, pa[:, :], w1e[es], w1e[es][:, kc, jc * 128:(jc + 1) * 128], h2T[bs], h2T[bs][:, kc, c0:c0 + 512], kc == 0, kc == 7)
                        pb = pab.nxt()
                        for kc in range(8):
                            k.mm(pb, pb[:, :], w3e[es], w3e[es][:, kc, jc * 128:(jc + 1) * 128], h2T[bs], h2T[bs][:, kc, c0:c0 + 512], kc == 0, kc == 7)
                        k.act(sl[jc % 2], sl[jc % 2][:], pa, pa[:, :], AF.Silu)
                        k.tt("dve", zT[zs], zT[zs][:, jc, :], sl[jc % 2], sl[jc % 2][:], pb, pb[:, :], ALU.mult)

                def stage_h(ui):
                    b_, ee, half = units[ui]
                    bs = b_ % 2
                    es = (b_ * 4 + ee) % 2
                    zs = ui % 2
                    for j in range(4):
                        tj = half * 4 + j
                        for hf in range(2):
                            pq = ph.nxt()
                            for jc in range(4):
                                k.mm(pq, pq[:, :], zT[zs], zT[zs][:, jc, j * 128:(j + 1) * 128],
                                     w2e[es], w2e[es][:, jc, hf * 512:(hf + 1) * 512], jc == 0, jc == 3)
                            oa = acc[:, tj, hf * 512:(hf + 1) * 512]
                            if ee == 0:
                                k.ts("dve", acc, oa, pq, pq[:, :], g4b[bs][:, tj, 0:1], None, ALU.mult, rd=[g4b[bs]])
                            else:
                                k.stt(acc, oa, pq, pq[:, :], g4b[bs][:, tj, ee:ee + 1], acc, oa, ALU.mult, ALU.add, rd=[g4b[bs]])
                    if ee == 3 and half == 1:
                        k.store("sp", YS, YS[b_ * 1024:(b_ + 1) * 1024, :].rearrange("(j p) f -> p j f", p=128), acc, acc[:])

                for ui in range(len(units) + 1):
                    if ui < len(units):
                        stage_a(ui)
                    if ui >= 1:
                        stage_h(ui - 1)
                    if ui < len(units):
                        b_, ee, half = units[ui]
                        if half == 0:
                            nb_, ne_ = (b_, ee + 1) if ee < 3 else (b_ + 1, 0)
                            if nb_ < NB:
                                load_w(nb_, ne_, 1)
                        if ee == 3 and half == 0 and b_ + 1 < NB:
                            transp_blk(b_ + 1)
                k.barrier()
            if stop_after == ("P7s", l):
                break

            with contextlib.ExitStack() as pst:
                k.phase_begin(pst)
                g2b = [k.sb("g2b%d" % i, [128, D], F32) for i in range(2)]
                for r in range(2):
                    k.load("sp", g2b[r], g2b[r][:], MOD[l, r:r + 1, 5 * D:6 * D].partition_broadcast(128), src=MOD)
                fnb = k.sb("fnb", [128, D], F32)
                k.load("sp", fnb, fnb[:], final_norm[0:1, :].partition_broadcast(128))
                NS8 = 6
                yg = [k.sb("yg%d" % i, [128, D], F32) for i in range(NS8)]
                xmt = [k.sb("xmt%d" % i, [128, D], F32) for i in range(NS8)]
                xo = [k.sb("xo%d" % i, [128, D], F32) for i in range(3)]
                junk = k.sb("junk8", [128, D], BF16)
                st8 = [k.sb("st8_%d" % i, [128, 8], F32) for i in range(3)]

                def fetch8(i_):
                    t = tiles_l[i_]
                    xs = i_ % NS8
                    k.dma("pool", yg[xs][:], YS[:, :], reads=[SLOTI, YS], writes=[yg[xs]], chan=k.bchan(yg[xs], "pool"), idx=("gather", SLOTI[:, t:t + 1]))
                    k.load("sp", xmt[xs], xmt[xs][:], XM.ap[t * 128:(t + 1) * 128, :], src=XM.g[gi_of_tile(t)])

                AH = 4
                for i_ in range(min(AH, len(tiles_l))):
                    fetch8(i_)
                for i_, t in enumerate(tiles_l):
                    xs = i_ % NS8
                    x3 = i_ % 3
                    g = gi_of_tile(t)
                    r = 1 if t < 2 else 0
                    if i_ + AH < len(tiles_l):
                        fetch8(i_ + AH)
                    k.tt("dve", xo[x3], xo[x3][:], yg[xs], yg[xs][:], g2b[r], g2b[r][:], ALU.mult)
                    k.tt("dve", xo[x3], xo[x3][:], xo[x3], xo[x3][:], xmt[xs], xmt[xs][:], ALU.add)
                    if l == 0:
                        k.store("sp", XR.g[g], XR.ap[t * 128:(t + 1) * 128, :], xo[x3], xo[x3][:])
                    else:
                        st = st8[x3]
                        k.act(junk, junk[:], xo[x3], xo[x3][:], AF.Square, accb=st, accum_out=st[:, 0:1])
                        k.act(st, st[:, 1:2], st, st[:, 0:1], AF.Ln, scale=1.0 / D, bias=EPS)
                        k.act(st, st[:, 2:3], st, st[:, 1:2], AF.Exp, scale=-0.5)
                        k.stt(xo[x3], xo[x3][:], xo[x3], xo[x3][:], st[:, 2:3], fnb, fnb[:], ALU.mult, ALU.mult, rd=[st])
                        k.store("sp", ybuf, y_out[(t - 2) * 128:(t - 1) * 128, :], xo[x3], xo[x3][:])
                k.barrier()
        k.barrier()
        final = {k.sem[e]: k.cnt[e] for e in k.ENGS}
        for c in k.chans + k.swchans + k.extra_chans:
            final[c.sem] = c.cnt
        for sem_, v in k.maxwait.items():
            assert v <= final[sem_], ("wait on never-reached semaphore value", sem_, v, final[sem_])
    return nc


def _ch(k, b):
    return k.bchan(b, "sp")


_CACHE = {}


def make_in_maps(inputs):
    x = np.asarray(inputs["x"], np.float32)
    B, S, _ = x.shape
    NX = S // 128
    hw = host_weights({k_: np.asarray(v, np.float32) for k_, v in inputs.items()})
    tabs = host_tables(NX)
    shared = dict(hw)
    shared.update(tabs)
    shared["ident"] = np.eye(128, dtype=np.float32)
    ctx = np.asarray(inputs["ctx"], np.float32)
    c = np.asarray(inputs["c"], np.float32)
    in_maps = []
    for b in range(B):
        m = dict(shared)
        m["x"] = np.ascontiguousarray(x[b])
        m["ctx"] = np.ascontiguousarray(ctx[b])
        m["c_t"] = _fm(c[b], 8)
        in_maps.append(m)
    return NX, in_maps


def kernel(**inputs):
    NX, in_maps = make_in_maps(inputs)
    B = len(in_maps)
    nc = build(NX)
    res = run_bass_kernel_spmd(nc, in_maps, core_ids=list(range(B)))
    return np.stack([np.asarray(r["y"], np.float32) for r in res.results], 0)
```

```python
import contextlib
import numpy as np
import concourse.bass as bass
import concourse.mybir as mybir
from concourse.bass_utils import run_bass_kernel_spmd

F32 = mybir.dt.float32
BF16 = mybir.dt.bfloat16
AF = mybir.ActivationFunctionType
ALU = mybir.AluOpType
AX = mybir.AxisListType

D = 1024
DEPTH = 2
CTX = 256
EPS = 1e-6
NE = 16
import os as _os
CUT = int(_os.environ.get("KCUT", "0"))
CA = dict(RQ=0, RQS=256, RK=512, RKS=768, RV=1024, PG=1536, CB=2048, CC=2560, CX=3072)
NCA = 3584
CBk = dict(QD=0, KVD=384, KR=640, KRS=672, GT=704)
NCB = 704 + 3072


class Buf:
    __slots__ = ("name", "w", "r", "t", "chan", "psum")

    def __init__(self, name, t=None):
        self.name = name
        self.w = {}
        self.r = {}
        self.t = t
        self.chan = None
        self.psum = False

    def __getitem__(self, k):
        return self.t[k]


class Chan:
    __slots__ = ("sem", "cnt")

    def __init__(self, sem):
        self.sem = sem
        self.cnt = 0


class KB:
    ENGS = ("pe", "act", "dve", "pool", "sp")

    def __init__(self, nc, stack, nchan=72, nsw=24):
        self.nc = nc
        self.gst = stack
        self.st = stack
        self.eng = {"pe": nc.tensor, "act": nc.scalar, "dve": nc.vector, "pool": nc.gpsimd, "sp": nc.sync}
        self.sem = {e: stack.enter_context(nc.semaphore("s_" + e)) for e in self.ENGS}
        self.cnt = {e: 0 for e in self.ENGS}
        self.seen = {e: {} for e in self.ENGS}
        self.chans = [Chan(stack.enter_context(nc.semaphore("c%d" % i))) for i in range(nchan)]
        self.swchans = [Chan(stack.enter_context(nc.semaphore("w%d" % i))) for i in range(nsw)]
        self.nextchan = 0
        self.nextsw = 0
        self.maxwait = {}
        self.nwait = 0
        self.nins = 0
        self.uid = 0

    def sb(self, name, shape, dt):
        self.uid += 1
        t = self.st.enter_context(self.nc.sbuf_tensor("%s_%d" % (name, self.uid), list(shape), dt))
        return Buf(name, t)

    def ps(self, name, shape, dt):
        self.uid += 1
        t = self.st.enter_context(self.nc.psum_tensor("%s_%d" % (name, self.uid), list(shape), dt))
        b = Buf(name, t)
        b.psum = True
        return b

    def dram(self, name, shape, dt, kind="Internal"):
        t = self.nc.dram_tensor(name, list(shape), dt, kind=kind)
        return Buf(name, t.ap())

    def chan(self, q="sp"):
        if q == "pool":
            c = self.swchans[self.nextsw]
            self.nextsw += 1
            return c
        c = self.chans[self.nextchan]
        self.nextchan += 1
        return c

    def bchan(self, b, q):
        if b.chan is None:
            b.chan = {}
        key = "sw" if q == "pool" else "hw"
        if key not in b.chan:
            b.chan[key] = self.chan(q)
        return b.chan[key]

    def phase_begin(self, stack):
        self.st = stack
        self.nextchan = 0
        self.nextsw = 0

    def _wait(self, e, need):
        seen = self.seen[e]
        eng = self.eng[e]
        for sem, val in need.items():
            if seen.get(sem, 0) >= val:
                continue
            eng.wait_ge(sem, val)
            seen[sem] = val
            self.nwait += 1
            if self.maxwait.get(sem, 0) < val:
                self.maxwait[sem] = val

    def op(self, e, fn, reads=(), writes=(), inc=True):
        mysem = self.sem[e]
        need = {}
        for b in reads:
            for s, v in b.w.items():
                if need.get(s, 0) < v:
                    need[s] = v
            if b.psum:
                for s, v in b.r.items():
                    if s is not mysem and need.get(s, 0) < v:
                        need[s] = v
        waw_self = (e != "pe")
        for b in writes:
            for s, v in b.w.items():
                if (s is not mysem or waw_self) and need.get(s, 0) < v:
                    need[s] = v
            for s, v in b.r.items():
                if (s is not mysem or waw_self) and need.get(s, 0) < v:
                    need[s] = v
        if need:
            self._wait(e, need)
        ins = fn()
        self.nins += 1
        if inc:
            self.cnt[e] += 1
            ins.then_inc(mysem, 1)
            tok = self.cnt[e]
        else:
            tok = self.cnt[e] + 1
        for b in reads:
            if b.r.get(mysem, 0) < tok:
                b.r[mysem] = tok
        for b in writes:
            b.w[mysem] = tok
        return ins

    def dma(self, q, out, in_, reads=(), writes=(), chan=None, **kw):
        need = {}
        for b in reads:
            for s, v in b.w.items():
                if need.get(s, 0) < v:
                    need[s] = v
        for b in writes:
            for s, v in b.w.items():
                if need.get(s, 0) < v:
                    need[s] = v
            for s, v in b.r.items():
                if need.get(s, 0) < v:
                    need[s] = v
        if need:
            self._wait(q, need)
        idx = kw.pop("idx", None)
        if idx is not None:
            mode, iap = idx
            off = bass.IndirectOffsetOnAxis(ap=iap, axis=0)
            if mode == "gather":
                ins = self.nc.gpsimd.indirect_dma_start(out=out, out_offset=None, in_=in_, in_offset=off, **kw)
            else:
                ins = self.nc.gpsimd.indirect_dma_start(out=out, out_offset=off, in_=in_, in_offset=None, **kw)
        else:
            ins = self.eng[q].dma_start(out=out, in_=in_, **kw)
        self.nins += 1
        chan.cnt += 16
        ins.then_inc(chan.sem, 16)
        tok = chan.cnt
        for b in reads:
            if b.r.get(chan.sem, 0) < tok:
                b.r[chan.sem] = tok
        for b in writes:
            b.w[chan.sem] = tok
        return ins

    def load(self, q, dst, dst_ap, src_ap, src=None, chan=None, **kw):
        if chan is None:
            chan = self.bchan(dst, q)
        return self.dma(q, dst_ap, src_ap, reads=([src] if src is not None else []), writes=[dst], chan=chan, **kw)

    def store(self, q, dst, dst_ap, src, src_ap, **kw):
        return self.dma(q, dst_ap, src_ap, reads=[src], writes=[dst], chan=self.bchan(src, q), **kw)

    def barrier(self):
        need = {self.sem[e]: self.cnt[e] for e in self.ENGS if self.cnt[e] > 0}
        for c in self.chans + self.swchans + getattr(self, "extra_chans", []):
            if c.cnt > 0:
                need[c.sem] = c.cnt
        for e in self.ENGS:
            n2 = {s: v for s, v in need.items() if s is not self.sem[e]}
            self._wait(e, n2)

    def mm(self, ob, o, lb, l, rb, r, start, stop, inc=None):
        nc = self.nc
        return self.op("pe", lambda: nc.tensor.matmul(o, l, r, start=start, stop=stop),
                       reads=[lb, rb], writes=[ob], inc=(stop if inc is None else inc))

    def tr(self, ob, o, ib, i, idb, idap, inc=True):
        nc = self.nc
        return self.op("pe", lambda: nc.tensor.transpose(o, i, idap), reads=[ib, idb], writes=[ob], inc=inc)

    def tt(self, e, ob, o, ab, a, bb, b, op):
        eng = self.eng[e]
        return self.op(e, lambda: eng.tensor_tensor(out=o, in0=a, in1=b, op=op), reads=[ab, bb], writes=[ob])

    def ts(self, e, ob, o, ab, a, s1, s2, op0, op1=None, rd=(), accum=None, accb=None):
        eng = self.eng[e]
        kw = {}
        if op1 is not None:
            kw["op1"] = op1
        if accum is not None:
            kw["accum_out"] = accum
        wr = [ob] + ([accb] if accb is not None else [])
        return self.op(e, lambda: eng.tensor_scalar(out=o, in0=a, scalar1=s1, scalar2=s2, op0=op0, **kw),
                       reads=[ab] + list(rd), writes=wr)

    def stt(self, ob, o, ab, a, sc, bb, b, op0, op1, rd=(), accum=None, accb=None):
        nc = self.nc
        kw = {}
        if accum is not None:
            kw["accum_out"] = accum
        wr = [ob] + ([accb] if accb is not None else [])
        return self.op("dve", lambda: nc.vector.scalar_tensor_tensor(out=o, in0=a, scalar=sc, in1=b, op0=op0, op1=op1, **kw),
                       reads=[ab, bb] + list(rd), writes=wr)

    def act(self, ob, o, ib, i, func, rd=(), accb=None, **kw):
        nc = self.nc
        wr = [ob] + ([accb] if accb is not None else [])
        return self.op("act", lambda: nc.scalar.activation(out=o, in_=i, func=func, **kw), reads=[ib] + list(rd), writes=wr)

    def cp(self, e, ob, o, ib, i):
        if e == "act":
            nc = self.nc
            return self.op("act", lambda: nc.scalar.copy(out=o, in_=i), reads=[ib], writes=[ob])
        eng = self.eng[e]
        return self.op(e, lambda: eng.tensor_copy(out=o, in_=i), reads=[ib], writes=[ob])

    def memset(self, e, ob, o, v):
        eng = self.eng[e]
        return self.op(e, lambda: eng.memset(o, v), writes=[ob])


def _fm(v, nch):
    return np.ascontiguousarray(np.swapaxes(v.reshape(v.shape[:-1] + (nch, 128)), -1, -2))


def host_tables(NX):
    T = CTX + NX * 128
    S = NX * 128
    pos = np.arange(S, dtype=np.float32)
    inv = (np.float32(10000.0) ** (-np.arange(32, dtype=np.float32) / np.float32(32))).astype(np.float32)
    ang = (pos[None, :] * inv[:, None]).astype(np.float32)
    c = np.cos(ang).astype(np.float32)
    s = np.sin(ang).astype(np.float32)
    cos64 = np.concatenate([c, c], 0)
    sin64 = np.concatenate([-s, s], 0)
    tabr = np.zeros((2, 128, T), np.float32)
    tabr[0, :, :CTX] = 1.0
    tabr[0, :, CTX:] = np.concatenate([cos64, cos64], 0)
    tabr[1, :, CTX:] = np.concatenate([sin64, sin64], 0)
    rows = (np.arange(S) // 64).astype(np.float32)
    cols = (np.arange(S) % 64).astype(np.float32)
    inv8 = (np.float32(10000.0) ** (-np.arange(8, dtype=np.float32) / np.float32(8))).astype(np.float32)
    ar = (rows[None, :] * inv8[:, None]).astype(np.float32)
    ac = (cols[None, :] * inv8[:, None]).astype(np.float32)
    cos32 = np.concatenate([np.cos(ar), np.cos(ar), np.cos(ac), np.cos(ac)], 0).astype(np.float32)
    sin32 = np.concatenate([-np.sin(ar), np.sin(ar), -np.sin(ac), np.sin(ac)], 0).astype(np.float32)
    tabm = np.zeros((2, 96, T), np.float32)
    tabm[0, :, :CTX] = 1.0
    tabm[0, :64, CTX:] = 1.0
    tabm[0, 64:, CTX:] = cos32
    tabm[1, 64:, CTX:] = sin32
    i = np.arange(128, dtype=np.float32)
    relf = np.maximum(i[None, :] - i[:, None], 0.0)
    relb = np.maximum(i[:, None] - i[None, :], 0.0)
    mskf = (i[None, :] >= i[:, None]).astype(np.float32)
    mskb = (i[:, None] > i[None, :]).astype(np.float32)
    rconst = np.stack([relf, relb, mskf, mskb], 0).astype(np.float32)
    xi = np.stack([np.broadcast_to(i[None, :] + 1.0, (128, 128)),
                   np.broadcast_to(128.0 - i[None, :], (128, 128))], 0).astype(np.float32)
    zt = np.stack([127.0 - i, i], 1).astype(np.float32)
    ustr = (i[:, None] < i[None, :]).astype(np.float32)
    mc = np.zeros((128, 64), np.float32)
    mc[:, 0:9] = np.arange(9, dtype=np.float32)[None, :] * 1024.0
    mc[:, 16:16 + 16] = np.arange(16, dtype=np.float32)[None, :] * 1024.0
    mc[:, 40:44] = np.arange(4, dtype=np.float32)[None, :] * 128.0 + i[:, None]
    return dict(tabr=tabr, tabm=tabm, rconst=rconst, xicst=xi, ztcst=np.ascontiguousarray(zt), ustr=ustr, mcst=mc)


def host_weights(inp):
    L = DEPTH
    w_in = inp["w_in"]
    offs = np.cumsum([0, 256, 256, 512, 512, 512, 512, 512, 384, 256, 32, 3072])
    o_rq, o_rk, o_rv, o_pg, o_cb, o_cc, o_cx, o_qd, o_kvd, o_kr, o_gt = offs[:11]
    sw64 = np.concatenate([np.arange(32, 64), np.arange(0, 32)])
    swq = np.concatenate([h * 64 + sw64 for h in range(4)])
    sw16 = np.concatenate([np.arange(8, 16), np.arange(0, 8)])
    sw32 = np.concatenate([sw16, 16 + sw16])
    idxa = np.concatenate([o_rq + np.arange(256), o_rq + swq, o_rk + np.arange(256), o_rk + swq,
                           o_rv + np.arange(512), o_pg + np.arange(512), o_cb + np.arange(512),
                           o_cc + np.arange(512), o_cx + np.arange(512)])
    idxb = np.concatenate([o_qd + np.arange(384), o_kvd + np.arange(256), o_kr + np.arange(32), o_kr + sw32,
                           o_gt + np.arange(3072)])
    assert idxa.size == NCA and idxb.size == NCB
    out = {}
    out["w_ina"] = np.ascontiguousarray(w_in[:, :, idxa])
    out["w_inb"] = np.ascontiguousarray(w_in[:, :, idxb])
    w_uq = inp["w_uq"].reshape(L, 384, 8, 96)
    wq = np.zeros((L, 384, 8, 2, 96), np.float32)
    wq[:, :, :, 0, :] = w_uq
    wq[:, :, :, 1, 64:] = w_uq[:, :, :, 64 + sw32]
    out["w_uqe"] = wq.reshape(L, 384, 8 * 2 * 96)
    w_ukv = inp["w_ukv"].reshape(L, 256, 8, 128)
    out["w_ukn"] = np.ascontiguousarray(w_ukv[:, :, :, :64]).reshape(L, 256, 512)
    out["w_ukvv"] = np.ascontiguousarray(w_ukv[:, :, :, 64:]).reshape(L, 256, 512)
    out["norm1_t"] = _fm(inp["norm1"], 8)
    out["norm2_t"] = _fm(inp["norm2"], 8)
    out["ret_gn_t"] = _fm(inp["ret_gn"], 4)
    out["qn_t"] = _fm(inp["mla_q_norm"], 3)
    out["kvn_t"] = _fm(inp["mla_kv_norm"], 2)
    out["convw_t"] = np.ascontiguousarray(inp["conv_w"].reshape(L, 4, 128, 3).transpose(0, 2, 1, 3))
    out["ret_decay"] = np.ascontiguousarray(inp["ret_decay"].reshape(L, 1, 8))
    for nm in ("w_ada", "b_ada", "w_ret_o", "w_conv_o", "w_mla_o", "w_out", "w_router"):
        out[nm] = np.ascontiguousarray(inp[nm])
    out["w1r"] = np.ascontiguousarray(inp["w1"].reshape(L, NE, 8, 128, 512).transpose(0, 1, 3, 2, 4)).reshape(L, NE * 128, 4096)
    out["w3r"] = np.ascontiguousarray(inp["w3"].reshape(L, NE, 8, 128, 512).transpose(0, 1, 3, 2, 4)).reshape(L, NE * 128, 4096)
    out["w2r"] = np.ascontiguousarray(inp["w2"].reshape(L, NE, 4, 128, 1024).transpose(0, 1, 3, 2, 4)).reshape(L, NE * 128, 4096)
    out["norm2_r"] = np.ascontiguousarray(inp["norm2"].reshape(L, 1, D))
    out["router_bias"] = np.ascontiguousarray(inp["router_bias"].reshape(1, NE))
    out["final_norm"] = np.ascontiguousarray(inp["final_norm"].reshape(1, D))
    out["cc_t"] = _fm(inp["c_ctx"], 8)
    return out


def build(NX, dbg=(), stop_after=None):
    T = CTX + NX * 128
    NT = T // 128
    groups = [(0, CTX)] + [(CTX + 512 * i, 512) for i in range(NX // 4)]
    NG = len(groups)
    nc = bass.Bass("TRN2", target_bir_lowering=False)
    L = DEPTH

    def knd(name):
        return "ExternalOutput" if name in dbg else "Internal"

    with contextlib.ExitStack() as gst:
        k = KB(nc, gst)
        I = lambda name, shape, dt=F32: k.dram(name, shape, dt, kind="ExternalInput")
        x_in = I("x", [NX * 128, D]); ctx_in = I("ctx", [CTX, D])
        c_t = I("c_t", [128, 8]); cc_t = I("cc_t", [128, 8])
        w_ada = I("w_ada", [L, D, 6 * D]); b_ada = I("b_ada", [L, 6 * D])
        norm1_t = I("norm1_t", [L, 128, 8]); norm2_t = I("norm2_t", [L, 128, 8])
        w_ina = I("w_ina", [L, D, NCA]); w_inb = I("w_inb", [L, D, NCB])
        ret_decay = I("ret_decay", [L, 1, 8]); ret_gn_t = I("ret_gn_t", [L, 128, 4])
        w_ret_o = I("w_ret_o", [L, 512, D]); convw_t = I("convw_t", [L, 128, 4, 3]); w_conv_o = I("w_conv_o", [L, 512, D])
        qn_t = I("qn_t", [L, 128, 3]); w_uqe = I("w_uqe", [L, 384, 1536]); kvn_t = I("kvn_t", [L, 128, 2])
        w_ukn = I("w_ukn", [L, 256, 512]); w_ukvv = I("w_ukvv", [L, 256, 512])
        w_mla_o = I("w_mla_o", [L, 512, D]); w_out = I("w_out", [L, D, D])
        w_router = I("w_router", [D, NE]); router_bias = I("router_bias", [1, NE])
        w1r = I("w1r", [L, NE * 128, 4096]); w3r = I("w3r", [L, NE * 128, 4096]); w2r = I("w2r", [L, NE * 128, 4096])
        norm2_r = I("norm2_r", [L, 1, D]); ustr_in = I("ustr", [128, 128]); mcst_in = I("mcst", [128, 64])
        final_norm = I("final_norm", [1, D])
        tabr = I("tabr", [2, 128, T]); tabm = I("tabm", [2, 96, T])
        rconst = I("rconst", [4, 128, 128]); xicst = I("xicst", [2, 128, 128]); ztcst = I("ztcst", [128, 2])
        ident_in = I("ident", [128, 128])
        ybuf = k.dram("y", [NX * 128, D], F32, kind="ExternalOutput")
        y_out = ybuf.t

        class Scr:
            def __init__(self, name, shape, dt, tok_axis):
                self.ap = nc.dram_tensor(name, list(shape), dt, kind=knd(name)).ap()
                self.g = [Buf("%s_g%d" % (name, i), None) for i in range(NG)]
                self.tok_axis = tok_axis

        MOD = k.dram("MOD", [L, 2, 6 * D], F32, kind=knd("MOD"))
        HT = Scr("HT", [8, 128, T], BF16, 2)
        RQ = Scr("RQ", [4, 128, T], BF16, 2)
        RKZ = Scr("RKZ", [T, 2, 256], BF16, 0)
        RV = Scr("RV", [T, 512], BF16, 0)
        PGS = Scr("PGS", [4, 128, T], BF16, 2)
        UU = Scr("UU", [4, 128, T], BF16, 2)
        BBs = Scr("BBs", [4, 128, T], BF16, 2)
        QT = Scr("QT", [8, 96, T], BF16, 2)
        KN = Scr("KN", [8, 64, T], BF16, 2)
        KR = Scr("KR", [32, T], BF16, 1)
        VV = Scr("VV", [8, T, 65], BF16, 1)
        GT = Scr("GT", [24, 128, T], BF16, 2)
        YR = Scr("YR", [8, 128, T], BF16, 2)
        YC = Scr("YC", [8, 128, T], BF16, 2)
        OT = Scr("OT", [4, 128, T], BF16, 2)
        XM = Scr("XM", [T, D], F32, 0)
        XR = Scr("XR", [T, D], F32, 0)
        H2T = Scr("H2T", [8, 128, T], BF16, 2)
        GATE = Scr("GATE", [T, NE], F32, 0)
        NB = T // 1024 + 4
        NS = NB * 1024
        I32 = mybir.dt.int32
        H2R = Scr("H2R", [T, D], BF16, 0)
        H2S = k.dram("H2S", [NS, D], BF16, kind=knd("H2S"))
        G4S = k.dram("G4S", [NS, 16], F32, kind=knd("G4S"))
        YS = k.dram("YS", [NS, D], F32, kind=knd("YS"))
        xsrc0 = [Buf("xin_g%d" % i) for i in range(NG)]

        def gi_of_tile(t):
            return 0 if t < 2 else 1 + (t - 2) // 4

        def res_src(l, t):
            if l == 0:
                if t < 2:
                    return ctx_in[t * 128:(t + 1) * 128, :], xsrc0[0]
                return x_in[(t - 2) * 128:(t - 1) * 128, :], xsrc0[gi_of_tile(t)]
            return XR.ap[t * 128:(t + 1) * 128, :], XR.g[gi_of_tile(t)]

        ident32 = k.sb("ident32", [128, 128], F32)
        identb = k.sb("identb", [128, 128], BF16)
        onesb = k.sb("onesb", [128, 128], BF16)
        mhalf = k.sb("mhalf", [128, 512], F32)
        modT = k.sb("modT", [128, L, 48, 2], F32)
        vecs = k.sb("vecs", [128, L, 2, 4, 8], F32)
        n1t = k.sb("n1t", [128, L, 8], F32); n2t = k.sb("n2t", [128, L, 8], F32)
        gnt = k.sb("gnt", [128, L, 4], F32); qnt = k.sb("qnt", [128, L, 3], F32); kvnt = k.sb("kvnt", [128, L, 2], F32)
        cwt = k.sb("cwt", [128, L, 4, 3], F32)
        rbias = k.sb("rbias", [128, NE], F32)
        rdec = k.sb("rdec", [128, L, 8], F32)
        lg = k.sb("lg", [128, L, 8], F32)
        gC = k.sb("gC", [128, L, 8], F32)
        zt = k.sb("zt", [128, L, 8], F32)
        ztc = k.sb("ztc", [128, 2], F32)
        G16A = k.sb("G16A", [128, NT, 16], F32)
        GSLA = k.sb("GSLA", [128, NT, 4], F32)
        RANKA = k.sb("RANKA", [128, NT], F32)
        SLOTI = k.sb("SLOTI", [128, NT], mybir.dt.int32)
        IDXW = k.sb("IDXW", [128, T // 1024 + 4, 4], mybir.dt.int32)
        carry = k.sb("carry", [128, 4], F32)
        ustrb = k.sb("ustrb", [128, 128], BF16)
        mc = k.sb("mc", [128, 64], F32)
        initc = k.chans.pop()
        initsw = k.swchans.pop()
        k.extra_chans = [initc, initsw]
        initbufs = []

        def ld(dst, src, q="sp", dap=None):
            k.dma(q, dst[:] if dap is None else dap, src, writes=[dst], chan=(initsw if q == "pool" else initc))
            initbufs.append(dst)
        ld(ident32, ident_in[:, :])
        ld(identb, ident_in[:, :], q="pool")
        ld(n1t, norm1_t[:, :, :].rearrange("l p c -> p l c")); ld(n2t, norm2_t[:, :, :].rearrange("l p c -> p l c"))
        ld(gnt, ret_gn_t[:, :, :].rearrange("l p c -> p l c")); ld(qnt, qn_t[:, :, :].rearrange("l p c -> p l c"))
        ld(kvnt, kvn_t[:, :, :].rearrange("l p c -> p l c")); ld(cwt, convw_t[:, :, :, :].rearrange("l p c j -> p l c j"))
        ld(rbias, router_bias[0:1, :].partition_broadcast(128))
        for l in range(L):
            ld(rdec, ret_decay[l, 0:1, :].partition_broadcast(128), dap=rdec[:, l, :])
        ld(ztc, ztcst[:, :])
        ld(mc, mcst_in[:, :])
        ld(ustrb, ustr_in[:, :], q="pool")
        k.memset("pool", G16A, G16A[:], 0.0)
        for b_ in initbufs:
            for c_ in (initc, initsw):
                if c_.sem in b_.w:
                    b_.w[c_.sem] = c_.cnt
        k.memset("pool", onesb, onesb[:], 1.0)
        k.memset("pool", mhalf, mhalf[:], -0.5)
        k.act(lg, lg[:], rdec, rdec[:], AF.Exp, scale=-1.0)
        k.act(lg, lg[:], lg, lg[:], AF.Ln, bias=1.0)
        k.ts("dve", lg, lg[:], lg, lg[:], -1.0, None, ALU.mult)
        k.act(gC, gC[:], lg, lg[:], AF.Exp, scale=128.0)
        for l in range(L):
            for h in range(4):
                k.act(zt, zt[:, l, h:h + 1], ztc, ztc[:, 0:1], AF.Exp, rd=[lg], scale=lg[:, l, h:h + 1])
                k.act(zt, zt[:, l, 4 + h:5 + h], ztc, ztc[:, 1:2], AF.Exp, rd=[lg], scale=lg[:, l, 4 + h:5 + h])

        with contextlib.ExitStack() as pst:
            k.phase_begin(pst)
            ct = k.sb("ct", [128, 2, 8], F32)
            sc = k.sb("sc", [128, 8, 2], F32)
            wa = [k.sb("wa%d" % i, [128, 8, 512], F32) for i in range(4)]
            bb = k.sb("bb", [2, 6 * D], F32)
            modrow = k.sb("modrow", [2, 6 * D], F32)
            pm = [k.ps("pm%d" % i, [128, 512], F32) for i in range(4)]
            pt = k.ps("pt", [128, 96], F32)
            k.load("sp", ct, ct[:, 0, :], c_t[:, :])
            k.load("sp", ct, ct[:, 1, :], cc_t[:, :])
            for r in range(2):
                k.act(sc, sc[:, :, r], ct, ct[:, r, :], AF.Silu)
            it = 0
            for l in range(L):
                k.load("sp", bb, bb[:], b_ada[l:l + 1, :].partition_broadcast(2))
                for n in range(12):
                    s = it % 4
                    k.load("sp", wa[s], wa[s][:], w_ada[l, :, n * 512:(n + 1) * 512].rearrange("(kc p) n -> p kc n", p=128))
                    for kc in range(8):
                        k.mm(pm[s], pm[s][0:2, :], sc, sc[:, kc, :], wa[s], wa[s][:, kc, :], kc == 0, kc == 7)
                    k.tt("dve", modrow, modrow[:, n * 512:(n + 1) * 512], pm[s], pm[s][0:2, :], bb, bb[:, n * 512:(n + 1) * 512], ALU.add)
                    it += 1
                k.store("sp", MOD, MOD[l, :, :], modrow, modrow[:])
                for j in range(48):
                    k.tr(pt, pt[:, 2 * j:2 * j + 2], modrow, modrow[0:2, j * 128:(j + 1) * 128], ident32, ident32[0:2, 0:2], inc=(j == 47))
                k.cp("dve", modT, modT[:, l, :, :], pt, pt[:, :].rearrange("p (j r) -> p j r", r=2))
                for r in range(2):
                    mv = lambda kk: modT[:, l, kk * 8:(kk + 1) * 8, r]
                    k.stt(vecs, vecs[:, l, r, 0, :], modT, mv(1), 1.0, n1t, n1t[:, l, :], ALU.add, ALU.mult)
                    k.cp("dve", vecs, vecs[:, l, r, 1, :], modT, mv(0))
                    k.stt(vecs, vecs[:, l, r, 2, :], modT, mv(4), 1.0, n2t, n2t[:, l, :], ALU.add, ALU.mult)
                    k.cp("dve", vecs, vecs[:, l, r, 3, :], modT, mv(3))
            k.barrier()

        class RR:
            def __init__(self, bufs):
                self.bufs = bufs
                self.i = 0

            def nxt(self):
                b = self.bufs[self.i % len(self.bufs)]
                self.i += 1
                return b

        def rsqrt_chain(dst, dst_ap, src, src_ap, mult, add, tmp, tmp_ap, mh_ap):
            k.ts("dve", tmp, tmp_ap, src, src_ap, mult, add, ALU.mult, ALU.add)
            k.op("pool", lambda: nc.gpsimd.tensor_tensor(out=dst_ap, in0=tmp_ap, in1=mh_ap, op=ALU.pow),
                 reads=[tmp, mhalf], writes=[dst])

        def rsqrt_big(dst, dst_ap, src, src_ap, mult, add, tmp, tmp_ap):
            k.act(tmp, tmp_ap, src, src_ap, AF.Ln, scale=mult, bias=add)
            k.act(dst, dst_ap, tmp, tmp_ap, AF.Exp, scale=-0.5)

        for l in range(L):
            with contextlib.ExitStack() as pst:
                k.phase_begin(pst)
                Wa = k.sb("Wa", [128, 8, NCA], BF16)
                for i in range(NCA // 512):
                    k.load("pool", Wa, Wa[:, :, i * 512:(i + 1) * 512],
                           w_ina[l, :, i * 512:(i + 1) * 512].rearrange("(kc p) n -> p kc n", p=128))
                xin = [k.sb("xin%d" % i, [128, D], F32) for i in range(2)]
                junk = k.sb("junk", [128, D], BF16)
                xn = [k.sb("xn%d" % i, [128, D], BF16) for i in range(2)]
                st1s = [k.sb("st1_%d" % i, [128, 8], F32) for i in range(2)]
                hTm = k.sb("hTm", [128, 8, 128], F32)
                hT = [k.sb("hT%d" % i, [128, 8, 512], BF16) for i in range(2)]
                trc = [k.sb("trc%d" % i, [128, 512], F32) for i in range(2)]
                trs = [k.sb("trs%d" % i, [128, 512], F32) for i in range(2)]
                s1 = [k.sb("s1_%d" % i, [128, 512], F32) for i in range(2)]
                s2 = [k.sb("s2_%d" % i, [128, 512], F32) for i in range(2)]
                qk = [k.sb("qk%d" % i, [128, 4, 512], BF16) for i in range(2)]
                pgs = [k.sb("pgs%d" % i, [128, 4, 512], BF16) for i in range(2)]
                uu = [k.sb("uu%d" % i, [128, 4, 512], BF16) for i in range(2)]
                bbs = [k.sb("bbs%d" % i, [128, 4, 512], BF16) for i in range(2)]
                csb = [k.sb("csb%d" % i, [128, 512], F32) for i in range(2)]
                rvs = [k.sb("rvs%d" % i, [128, 4, 512], BF16) for i in range(2)]
                kz = [k.sb("kz%d" % i, [128, 4, 2, 256], BF16) for i in range(2)]
                pp = RR([k.ps("pp%d" % i, [128, 512], F32) for i in range(6)])
                ptr = k.ps("ptr", [128, 8, 128], BF16)
                ptk = k.ps("ptk", [128, 256], BF16)
                ti = 0
                for g, (t0, n) in enumerate(groups):
                    gs = g % 2
                    r = 1 if g == 0 else 0
                    ntile = n // 128
                    k.load("sp", trc[gs], trc[gs][:, 0:n], tabr[0, :, t0:t0 + n])
                    k.load("sp", trs[gs], trs[gs][:, 0:n], tabr[1, :, t0:t0 + n])
                    for j in range(ntile):
                        t = t0 // 128 + j
                        xs = ti % 2
                        ti += 1
                        sap, sbuf_ = res_src(l, t)
                        st1 = st1s[xs]
                        k.load("sp", xin[xs], xin[xs][:], sap, src=sbuf_)
                        k.act(junk, junk[:], xin[xs], xin[xs][:], AF.Square, accb=st1, accum_out=st1[:, 0:1])
                        rsqrt_chain(st1, st1[:, 2:3], st1, st1[:, 0:1], 1.0 / D, EPS, st1, st1[:, 1:2], mhalf[:, 0:1])
                        k.ts("dve", xn[xs], xn[xs][:], xin[xs], xin[xs][:], st1[:, 2:3], None, ALU.mult, rd=[st1])
                        for c in range(8):
                            k.tr(ptr, ptr[:, c, :], xn[xs], xn[xs][:, c * 128:(c + 1) * 128], identb, identb[:], inc=(c == 7))
                        k.tt("dve", hTm, hTm[:], ptr, ptr[:], vecs, vecs[:, l, r, 0, :].unsqueeze(2).to_broadcast([128, 8, 128]), ALU.mult)
                        k.tt("dve", hT[gs], hT[gs][:, :, j * 128:(j + 1) * 128], hTm, hTm[:],
                             vecs, vecs[:, l, r, 1, :].unsqueeze(2).to_broadcast([128, 8, 128]), ALU.add)
                    k.store("sp", HT.g[g], HT.ap[:, :, t0:t0 + n].rearrange("c p t -> p c t"), hT[gs], hT[gs][:, :, 0:n])

                    def proj(col0, M):
                        p = pp.nxt()
                        for kc in range(8):
                            k.mm(p, p[0:M, 0:n], Wa, Wa[:, kc, col0:col0 + M], hT[gs], hT[gs][:, kc, 0:n], kc == 0, kc == 7)
                        return p

                    for pr in range(2):
                        pa = proj(CA["RQ"] + pr * 128, 128)
                        pb = proj(CA["RQS"] + pr * 128, 128)
                        k.tt("dve", s1[pr], s1[pr][:, 0:n], pa, pa[:, 0:n], trc[gs], trc[gs][:, 0:n], ALU.mult)
                        k.tt("dve", s2[pr], s2[pr][:, 0:n], pb, pb[:, 0:n], trs[gs], trs[gs][:, 0:n], ALU.mult)
                        k.tt("pool", qk[gs], qk[gs][:, pr, 0:n], s1[pr], s1[pr][:, 0:n], s2[pr], s2[pr][:, 0:n], ALU.add)
                    for pr in range(2):
                        pa = proj(CA["RK"] + pr * 128, 128)
                        pb = proj(CA["RKS"] + pr * 128, 128)
                        k.stt(s1[pr], s1[pr][:, 0:n], pa, pa[:, 0:n], 0.125, trc[gs], trc[gs][:, 0:n], ALU.mult, ALU.mult)
                        k.stt(s2[pr], s2[pr][:, 0:n], pb, pb[:, 0:n], 0.125, trs[gs], trs[gs][:, 0:n], ALU.mult, ALU.mult)
                        k.tt("pool", qk[gs], qk[gs][:, 2 + pr, 0:n], s1[pr], s1[pr][:, 0:n], s2[pr], s2[pr][:, 0:n], ALU.add)
                    k.store("sp", RQ.g[g], RQ.ap[:, :, t0:t0 + n].rearrange("c p t -> p c t"), qk[gs], qk[gs][:, :, 0:n])
                    for c in range(4):
                        p = proj(CA["PG"] + c * 128, 128)
                        k.act(pgs[gs], pgs[gs][:, c, 0:n], p, p[:, 0:n], AF.Silu)
                    k.store("sp", PGS.g[g], PGS.ap[:, :, t0:t0 + n].rearrange("c p t -> p c t"), pgs[gs], pgs[gs][:, :, 0:n])
                    for c in range(4):
                        p = proj(CA["CB"] + c * 128, 128)
                        k.cp("act", bbs[gs], bbs[gs][:, c, 0:n], p, p[:, 0:n])
                    k.store("sp", BBs.g[g], BBs.ap[:, :, t0:t0 + n].rearrange("c p t -> p c t"), bbs[gs], bbs[gs][:, :, 0:n])
                    for c in range(4):
                        pc = proj(CA["CC"] + c * 128, 128)
                        px = proj(CA["CX"] + c * 128, 128)
                        k.cp("act", csb[c % 2], csb[c % 2][:, 0:n], pc, pc[:, 0:n])
                        k.tt("dve", uu[gs], uu[gs][:, c, 0:n], px, px[:, 0:n], csb[c % 2], csb[c % 2][:, 0:n], ALU.mult)
                    k.store("sp", UU.g[g], UU.ap[:, :, t0:t0 + n].rearrange("c p t -> p c t"), uu[gs], uu[gs][:, :, 0:n])
                    for j in range(ntile):
                        p = pp.nxt()
                        for kc in range(8):
                            k.mm(p, p[:, :], hT[gs], hT[gs][:, kc, j * 128:(j + 1) * 128], Wa, Wa[:, kc, CA["RV"]:CA["RV"] + 512], kc == 0, kc == 7)
                        k.cp("act", rvs[gs], rvs[gs][:, j, :], p, p[:, :])
                        for pr in range(2):
                            k.tr(ptk, ptk[:, pr * 128:(pr + 1) * 128], qk[gs], qk[gs][:, 2 + pr, j * 128:(j + 1) * 128], identb, identb[:], inc=(pr == 1))
                        for d_ in range(2):
                            k.tt("dve", kz[gs], kz[gs][:, j, d_, :].rearrange("p (h d) -> p h d", h=4),
                                 ptk, ptk[:, :].rearrange("p (h d) -> p h d", h=4),
                                 zt, zt[:, l, d_ * 4:(d_ + 1) * 4].unsqueeze(2).to_broadcast([128, 4, 64]), ALU.mult)
                    k.store("sp", RV.g[g], RV.ap[t0:t0 + n, :].rearrange("(j p) c -> p j c", p=128), rvs[gs], rvs[gs][:, 0:ntile, :])
                    k.store("sp", RKZ.g[g], RKZ.ap[t0:t0 + n, :, :].rearrange("(j p) d c -> p j d c", p=128), kz[gs], kz[gs][:, 0:ntile, :, :])
                k.barrier()
            if stop_after == ("P1a", l):
                break

            with contextlib.ExitStack() as pst:
                k.phase_begin(pst)
                Wb = k.sb("Wb", [128, 8, NCB], BF16)
                PW = NCB // 8
                for i in range(8):
                    k.load("pool", Wb, Wb[:, :, i * PW:(i + 1) * PW],
                           w_inb[l, :, i * PW:(i + 1) * PW].rearrange("(kc p) n -> p kc n", p=128))
                wuq = k.sb("wuq", [128, 3, 1536], BF16)
                for c in range(3):
                    k.load("pool", wuq, wuq[:, c, :], w_uqe[l, c * 128:(c + 1) * 128, :])
                wkn = k.sb("wkn", [128, 2, 512], BF16)
                wvv = k.sb("wvv", [128, 2, 512], BF16)
                k.load("pool", wkn, wkn[:], w_ukn[l, :, :].rearrange("(kc p) n -> p kc n", p=128))
                k.load("pool", wvv, wvv[:], w_ukvv[l, :, :].rearrange("(kc p) n -> p kc n", p=128))
                hT = [k.sb("hTb%d" % i, [128, 8, 512], BF16) for i in range(2)]
                c96 = k.sb("c96", [96, 512], F32); s96 = k.sb("s96", [96, 512], F32)
                c32 = k.sb("c32", [32, 512], F32); s32 = k.sb("s32", [32, 512], F32)
                sqq = k.sb("sqq", [128, 3, 512], BF16); pqn = k.sb("pqn", [128, 3, 512], BF16)
                sqkv = k.sb("sqkv", [128, 2, 512], BF16); pkvn = k.sb("pkvn", [128, 2, 512], BF16)
                rtmp = k.sb("rtmp", [128, 512], F32)
                rstdq = k.sb("rstdq", [128, 512], F32); rkvb = k.sb("rkvb", [128, 512], F32)
                rkvt = k.sb("rkvt", [128, 8], F32)
                s1 = [k.sb("b_s1_%d" % i, [128, 512], F32) for i in range(2)]
                s2 = [k.sb("b_s2_%d" % i, [128, 512], F32) for i in range(2)]
                krs = k.sb("krs", [32, 512], BF16)
                qts = k.sb("qts", [96, 8, 512], BF16)
                kns = k.sb("kns", [64, 8, 512], BF16)
                vs = k.sb("vs", [128, 4, 8, 65], BF16)
                gts = [k.sb("gts%d" % i, [128, 6, 512], BF16) for i in range(2)]
                pp = RR([k.ps("ppb%d" % i, [128, 512], F32) for i in range(7)])
                pst4 = k.ps("pst4", [128, 4], F32)
                k.memset("pool", vs, vs[:], 1.0)
                for g, (t0, n) in enumerate(groups):
                    gs = g % 2
                    ntile = n // 128
                    if g == 0:
                        k.load("sp", hT[0], hT[0][:, :, 0:n], HT.ap[:, :, t0:t0 + n].rearrange("c p t -> p c t"), src=HT.g[0])
                    if g + 1 < NG:
                        t0n, nn = groups[g + 1]
                        k.load("sp", hT[(g + 1) % 2], hT[(g + 1) % 2][:, :, 0:nn], HT.ap[:, :, t0n:t0n + nn].rearrange("c p t -> p c t"), src=HT.g[g + 1])
                    k.load("sp", c96, c96[:, 0:n], tabm[0, :, t0:t0 + n]); k.load("sp", s96, s96[:, 0:n], tabm[1, :, t0:t0 + n])
                    k.load("sp", c32, c32[:, 0:n], tabm[0, 64:96, t0:t0 + n]); k.load("sp", s32, s32[:, 0:n], tabm[1, 64:96, t0:t0 + n])

                    if CUT == 11:
                        continue

                    def proj(col0, M):
                        p = pp.nxt()
                        for kc in range(8):
                            k.mm(p, p[0:M, 0:n], Wb, Wb[:, kc, col0:col0 + M], hT[gs], hT[gs][:, kc, 0:n], kc == 0, kc == 7)
                        return p

                    for c in range(3):
                        p = proj(CBk["QD"] + c * 128, 128)
                        if CUT != 122:
                            k.act(sqq, sqq[:, c, 0:n], p, p[:, 0:n], AF.Square)
                        if CUT != 121:
                            k.ts("dve", pqn, pqn[:, c, 0:n], p, p[:, 0:n], qnt[:, l, c:c + 1], None, ALU.mult, rd=[qnt])
                    for c in range(2):
                        p = proj(CBk["KVD"] + c * 128, 128)
                        if CUT != 122:
                            k.act(sqkv, sqkv[:, c, 0:n], p, p[:, 0:n], AF.Square)
                        if CUT != 121:
                            k.ts("dve", pkvn, pkvn[:, c, 0:n], p, p[:, 0:n], kvnt[:, l, c:c + 1], None, ALU.mult, rd=[kvnt])
                    if CUT in (12, 121, 122):
                        continue
                    p = pp.nxt()
                    for c in range(3):
                        k.mm(p, p[:, 0:n], onesb, onesb[:], sqq, sqq[:, c, 0:n], c == 0, c == 2)
                    rsqrt_big(rstdq, rstdq[:, 0:n], p, p[:, 0:n], 96.0 / 384.0, 96.0 * EPS, rtmp, rtmp[:, 0:n])
                    p = pp.nxt()
                    for c in range(2):
                        k.mm(p, p[:, 0:n], onesb, onesb[:], sqkv, sqkv[:, c, 0:n], c == 0, c == 1)
                    rsqrt_big(rkvb, rkvb[:, 0:n], p, p[:, 0:n], 1.0 / 256.0, EPS, rtmp, rtmp[:, 0:n])
                    if CUT == 13:
                        continue
                    for j in range(ntile):
                        for c in range(2):
                            k.mm(pst4, pst4[:, j:j + 1], sqkv, sqkv[:, c, j * 128:(j + 1) * 128], onesb, onesb[:, 0:1], c == 0, c == 1)
                    rsqrt_chain(rkvt, rkvt[:, 4:4 + ntile], pst4, pst4[:, 0:ntile], 1.0 / 256.0, EPS, rkvt, rkvt[:, 0:ntile], mhalf[:, 0:ntile])
                    if CUT == 1:
                        continue
                    pa = proj(CBk["KR"], 32)
                    pb = proj(CBk["KRS"], 32)
                    k.tt("dve", s1[0], s1[0][0:32, 0:n], pa, pa[0:32, 0:n], c32, c32[:, 0:n], ALU.mult)
                    k.tt("dve", s2[0], s2[0][0:32, 0:n], pb, pb[0:32, 0:n], s32, s32[:, 0:n], ALU.mult)
                    k.tt("pool", krs, krs[:, 0:n], s1[0], s1[0][0:32, 0:n], s2[0], s2[0][0:32, 0:n], ALU.add)
                    k.store("sp", KR.g[g], KR.ap[:, t0:t0 + n], krs, krs[:, 0:n])
                    if CUT == 2:
                        continue
                    for h in range(8):
                        hs = h % 2
                        pa = pp.nxt()
                        for c in range(3):
                            k.mm(pa, pa[0:96, 0:n], wuq, wuq[:, c, (2 * h) * 96:(2 * h + 1) * 96], pqn, pqn[:, c, 0:n], c == 0, c == 2)
                        pb = pp.nxt()
                        for c in range(3):
                            k.mm(pb, pb[0:96, 0:n], wuq, wuq[:, c, (2 * h + 1) * 96:(2 * h + 2) * 96], pqn, pqn[:, c, 0:n], c == 0, c == 2)
                        k.tt("dve", s1[hs], s1[hs][0:96, 0:n], pa, pa[0:96, 0:n], c96, c96[:, 0:n], ALU.mult)
                        k.tt("dve", s2[hs], s2[hs][0:96, 0:n], pb, pb[0:96, 0:n], s96, s96[:, 0:n], ALU.mult)
                        k.tt("pool", s1[hs], s1[hs][0:96, 0:n], s1[hs], s1[hs][0:96, 0:n], s2[hs], s2[hs][0:96, 0:n], ALU.add)
                        k.tt("pool", qts, qts[:, h, 0:n], s1[hs], s1[hs][0:96, 0:n], rstdq, rstdq[0:96, 0:n], ALU.mult)
                        pk_ = pp.nxt()
                        for c in range(2):
                            k.mm(pk_, pk_[0:64, 0:n], wkn, wkn[:, c, h * 64:(h + 1) * 64], pkvn, pkvn[:, c, 0:n], c == 0, c == 1)
                        k.tt("dve", kns, kns[:, h, 0:n], pk_, pk_[0:64, 0:n], rkvb, rkvb[0:64, 0:n], ALU.mult)
                    k.store("sp", QT.g[g], QT.ap[:, :, t0:t0 + n].rearrange("h r t -> r h t"), qts, qts[:, :, 0:n])
                    k.store("sp", KN.g[g], KN.ap[:, :, t0:t0 + n].rearrange("h r t -> r h t"), kns, kns[:, :, 0:n])
                    if CUT == 3:
                        continue
                    for j in range(ntile):
                        p = pp.nxt()
                        for c in range(2):
                            k.mm(p, p[:, :], pkvn, pkvn[:, c, j * 128:(j + 1) * 128], wvv, wvv[:, c, :], c == 0, c == 1)
                        k.ts("dve", vs, vs[:, j, :, 0:64], p, p[:, :].rearrange("p (h e) -> p h e", h=8), rkvt[:, 4 + j:5 + j], None, ALU.mult, rd=[rkvt])
                    for j in range(ntile):
                        k.store("sp", VV.g[g], VV.ap[:, t0 + j * 128:t0 + (j + 1) * 128, :].rearrange("h p e -> p h e"), vs, vs[:, j, :, :])
                    if CUT == 4:
                        continue
                    for c6 in range(4):
                        gb = gts[c6 % 2]
                        for cc in range(6):
                            c = c6 * 6 + cc
                            p = proj(CBk["GT"] + c * 128, 128)
                            k.act(gb, gb[:, cc, 0:n], p, p[:, 0:n], AF.Sigmoid)
                        k.store("sp", GT.g[g], GT.ap[c6 * 6:(c6 + 1) * 6, :, t0:t0 + n].rearrange("c p t -> p c t"), gb, gb[:, :, 0:n])
                k.barrier()
            if stop_after == ("P1b", l):
                break

            with contextlib.ExitStack() as pst:
                k.phase_begin(pst)
                rc = k.sb("rc", [128, 4, 128], F32)
                xic = k.sb("xic", [128, 2, 128], F32)
                k.load("sp", rc, rc[:], rconst[:, :, :].rearrange("a p i -> p a i"))
                k.load("sp", xic, xic[:], xicst[:, :, :].rearrange("a p i -> p a i"))
                Mdec = k.sb("Mdec", [128, 4, 128], F32)
                etmp = k.sb("etmp", [128, 2, 128], F32)
                TAB3 = k.sb("TAB3", [128, 3, 4, 128], F32)
                gCp = k.sb("gCp", [128, 2, 2], F32)
                k.memset("pool", TAB3, TAB3[:], 0.0)
                for h in range(4):
                    pr, hh = h // 2, h % 2
                    k.act(etmp, etmp[:, 0, :], rc, rc[:, 0, :], AF.Exp, rd=[lg], scale=lg[:, l, h:h + 1])
                    k.act(etmp, etmp[:, 1, :], rc, rc[:, 1, :], AF.Exp, rd=[lg], scale=lg[:, l, 4 + h:5 + h])
                    k.tt("dve", etmp, etmp[:], etmp, etmp[:], rc, rc[:, 2:4, :], ALU.mult)
                    k.tt("dve", Mdec, Mdec[:, h, :], etmp, etmp[:, 0, :], etmp, etmp[:, 1, :], ALU.add)
                    ps_ = slice(hh * 64, (hh + 1) * 64)
                    k.memset("dve", TAB3, TAB3[ps_, 0, h, :], 1.0)
                    k.act(TAB3, TAB3[ps_, 1, h, :], xic, xic[ps_, 0, :], AF.Exp, rd=[lg], scale=lg[ps_, l, h:h + 1])
                    k.act(TAB3, TAB3[ps_, 2, h, :], xic, xic[ps_, 1, :], AF.Exp, rd=[lg], scale=lg[ps_, l, 4 + h:5 + h])
                    for d_ in range(2):
                        k.cp("dve", gCp, gCp[ps_, d_, pr:pr + 1], gC, gC[ps_, l, d_ * 4 + h:d_ * 4 + h + 1])
                wrof = k.sb("wrof", [128, 4, D], F32)
                wro = k.sb("wro", [128, 4, D], BF16)
                k.load("sp", wrof, wrof[:], w_ret_o[l, :, :].rearrange("(h p) f -> p h f", p=128))
                for h in range(4):
                    k.ts("pool", wro, wro[:, h, :], wrof, wrof[:, h, :], gnt[:, l, h:h + 1], None, ALU.mult, rd=[gnt])
                SBprev = k.sb("SBprev", [128, NT, 2, 128], BF16)
                Sf = k.sb("Sf", [128, 2, 128], F32); Sb = k.sb("Sb", [128, 2, 128], F32)
                Sfb = [k.sb("Sfb%d" % i, [128, 2, 128], BF16) for i in range(2)]
                qkg = [k.sb("qkg%d" % i, [128, 4, 512], BF16) for i in range(2)]
                kzg = [k.sb("kzg%d" % i, [128, 4, 2, 256], BF16) for i in range(2)]
                rvg = [k.sb("rvg%d" % i, [128, 4, 512], BF16) for i in range(2)]
                pgg = [k.sb("pgg%d" % i, [128, 4, 512], BF16) for i in range(2)]
                kzb = [k.sb("kzb%d" % i, [128, 4, 256], BF16) for i in range(2)]
                rvb = [k.sb("rvb%d" % i, [128, 4, 512], BF16) for i in range(2)]
                Q3 = [k.sb("Q3_%d" % i, [128, 3, 4, 128], BF16) for i in range(2)]
                ms = [k.sb("ms%d" % i, [128, 4, 128], BF16) for i in range(2)]
                ysq_ = [k.sb("ysq%d" % i, [128, 512], BF16) for i in range(2)]
                rst_ = [k.sb("rst%d" % i, [128, 512], F32) for i in range(2)]
                rtm_ = [k.sb("rtm%d" % i, [128, 512], F32) for i in range(2)]
                ytm_ = [k.sb("ytm%d" % i, [128, 512], F32) for i in range(2)]
                rTg = [k.sb("rTg%d" % i, [128, 4, 512], BF16) for i in range(2)]
                yrs = [k.sb("yrs%d" % i, [128, 8, 512], BF16) for i in range(2)]
                psc = [k.ps("psc%d" % i, [128, 512], F32) for i in range(1)] * 2
                py = [k.ps("py%d" % i, [128, 512], F32) for i in range(2)]
                pu = [k.ps("pu%d" % i, [128, 512], F32) for i in range(2)]
                pssq2 = [k.ps("pssq%d" % i, [128, 512], F32) for i in range(2)]
                pyr = k.ps("pyr", [128, 512], F32)
                k.memset("dve", Sf, Sf[:], 0.0); k.memset("dve", Sb, Sb[:], 0.0)
                k.memset("pool", Sfb[0], Sfb[0][:], 0.0)

                def s_update(S, dir_, pub):
                    for pr in range(2):
                        for hh in range(2):
                            ps_ = slice(hh * 64, (hh + 1) * 64)
                            k.stt(S, S[ps_, pr, :], S, S[ps_, pr, :], gCp[ps_, dir_, pr:pr + 1], pub,
                                  pub[ps_, pr * 256 + hh * 128:pr * 256 + (hh + 1) * 128], ALU.mult, ALU.add, rd=[gCp])

                it = 0
                for g in [0] + list(range(NG - 1, 0, -1)):
                    t0, n = groups[g]
                    ntile = n // 128
                    gs = it % 2
                    it += 1
                    k.load("sp", kzb[gs], kzb[gs][:, 0:ntile, :], RKZ.ap[t0:t0 + n, 1, :].rearrange("(j p) c -> p j c", p=128), src=RKZ.g[g])
                    k.load("sp", rvb[gs], rvb[gs][:, 0:ntile, :], RV.ap[t0:t0 + n, :].rearrange("(j p) c -> p j c", p=128), src=RV.g[g])
                    for j in range(ntile - 1, -1, -1):
                        t = t0 // 128 + j
                        k.cp("act", SBprev, SBprev[:, t, :, :], Sb, Sb[:])
                        pub = pu[t % 2]
                        for pr in range(2):
                            k.mm(pub, pub[:, pr * 256:(pr + 1) * 256], kzb[gs], kzb[gs][:, j, pr * 128:(pr + 1) * 128],
                                 rvb[gs], rvb[gs][:, j, pr * 256:(pr + 1) * 256], True, True, inc=(pr == 1))
                        s_update(Sb, 1, pub)

                def stageA(t):
                    g = gi_of_tile(t)
                    t0, n = groups[g]
                    j = t - t0 // 128
                    gs = g % 2
                    if j == 0:
                        ntile = n // 128
                        k.load("sp", qkg[gs], qkg[gs][:, :, 0:n], RQ.ap[:, :, t0:t0 + n].rearrange("c p t -> p c t"), src=RQ.g[g])
                        k.load("sp", kzg[gs], kzg[gs][:, 0:ntile, :, :], RKZ.ap[t0:t0 + n, :, :].rearrange("(j p) d c -> p j d c", p=128), src=RKZ.g[g])
                        k.load("sp", rvg[gs], rvg[gs][:, 0:ntile, :], RV.ap[t0:t0 + n, :].rearrange("(j p) c -> p j c", p=128), src=RV.g[g])
                        k.load("sp", pgg[gs], pgg[gs][:, :, 0:n], PGS.ap[:, :, t0:t0 + n].rearrange("c p t -> p c t"), src=PGS.g[g])
                    cs = slice(j * 128, (j + 1) * 128)
                    q3 = Q3[t % 2]
                    for pr in range(2):
                        k.tt("dve", q3, q3[:, :, 2 * pr:2 * pr + 2, :], qkg[gs],
                             qkg[gs][:, pr, cs].unsqueeze(1).unsqueeze(1).to_broadcast([128, 3, 2, 128]),
                             TAB3, TAB3[:, :, 2 * pr:2 * pr + 2, :], ALU.mult)
                    pscb = psc[t % 2]
                    for h in range(4):
                        k.mm(pscb, pscb[:, h * 128:(h + 1) * 128], qkg[gs], qkg[gs][:, 2 + h // 2, cs], q3, q3[:, 0, h, :], True, True, inc=(h == 3))
                    k.tt("dve", ms[t % 2], ms[t % 2][:], pscb, pscb[:, :].rearrange("p (h i) -> p h i", h=4), Mdec, Mdec[:], ALU.mult)

                def stageB(t):
                    g = gi_of_tile(t)
                    t0, n = groups[g]
                    j = t - t0 // 128
                    gs = g % 2
                    cs = slice(j * 128, (j + 1) * 128)
                    q3 = Q3[t % 2]
                    pyb = py[t % 2]
                    sfb = Sfb[t % 2]
                    for h in range(4):
                        pr = h // 2
                        o = pyb[:, h * 128:(h + 1) * 128]
                        k.mm(pyb, o, rvg[gs], rvg[gs][:, j, h * 128:(h + 1) * 128], ms[t % 2], ms[t % 2][:, h, :], True, False)
                        k.mm(pyb, o, sfb, sfb[:, pr, :], q3, q3[:, 1, h, :], False, False)
                        k.mm(pyb, o, SBprev, SBprev[:, t, pr, :], q3, q3[:, 2, h, :], False, True, inc=(h == 3))
                    pub = pu[t % 2]
                    for pr in range(2):
                        k.mm(pub, pub[:, pr * 256:(pr + 1) * 256], kzg[gs], kzg[gs][:, j, 0, pr * 128:(pr + 1) * 128],
                             rvg[gs], rvg[gs][:, j, pr * 256:(pr + 1) * 256], True, True, inc=(pr == 1))
                    s_update(Sf, 0, pub)
                    k.cp("act", Sfb[(t + 1) % 2], Sfb[(t + 1) % 2][:], Sf, Sf[:])
                    ysq = ysq_[t % 2]
                    k.act(ysq, ysq[:], pyb, pyb[:], AF.Square)
                    k.mm(pssq2[t % 2], pssq2[t % 2][:], onesb, onesb[:], ysq, ysq[:], True, True)

                def stageC(t):
                    g = gi_of_tile(t)
                    t0, n = groups[g]
                    j = t - t0 // 128
                    gs = g % 2
                    cs = slice(j * 128, (j + 1) * 128)
                    pyb = py[t % 2]
                    rst = rst_[t % 2]; rtm = rtm_[t % 2]; ytm = ytm_[t % 2]; pssq = pssq2[t % 2]
                    rsqrt_big(rst, rst[:], pssq, pssq[:], 1.0 / 128.0, EPS, rtm, rtm[:])
                    k.tt("dve", ytm, ytm[:], pyb, pyb[:], rst, rst[:], ALU.mult)
                    k.tt("pool", rTg[gs], rTg[gs][:, :, cs], ytm, ytm[:].rearrange("p (h i) -> p h i", h=4), pgg[gs], pgg[gs][:, :, cs], ALU.mult)
                    if j == n // 128 - 1:
                        for fc in range(8):
                            for h in range(4):
                                k.mm(pyr, pyr[:, 0:n], wro, wro[:, h, fc * 128:(fc + 1) * 128], rTg[gs], rTg[gs][:, h, 0:n], h == 0, h == 3)
                            k.cp("act", yrs[gs], yrs[gs][:, fc, 0:n], pyr, pyr[:, 0:n])
                        k.store("sp", YR.g[g], YR.ap[:, :, t0:t0 + n].rearrange("c p t -> p c t"), yrs[gs], yrs[gs][:, :, 0:n])

                stageA(0)
                for idx in range(1, NT + 2):
                    if idx - 1 < NT:
                        stageB(idx - 1)
                    if idx < NT:
                        stageA(idx)
                    if idx >= 2:
                        stageC(idx - 2)
                k.barrier()
            if stop_after == ("P3", l):
                break

            with contextlib.ExitStack() as pst:
                k.phase_begin(pst)
                wco = k.sb("wco", [128, 4, D], BF16)
                k.load("pool", wco, wco[:], w_conv_o[l, :, :].rearrange("(c p) f -> p c f", p=128))
                ug = [k.sb("ug%d" % i, [128, 4, 514], BF16) for i in range(2)]
                bg = [k.sb("bg%d" % i, [128, 4, 512], BF16) for i in range(2)]
                cv = [k.sb("cv%d" % i, [128, 512], F32) for i in range(2)]
                cT = [k.sb("cT%d" % i, [128, 4, 512], BF16) for i in range(2)]
                ycs = [k.sb("ycs%d" % i, [128, 8, 512], BF16) for i in range(2)]
                pc_ = RR([k.ps("pcv%d" % i, [128, 512], F32) for i in range(4)])
                def load4(g):
                    t0, n = groups[g]
                    gs = g % 2
                    first = (g == 0) or (g == 1)
                    last = (g == 0) or (g == NG - 1)
                    lo = t0 if first else t0 - 1
                    hi = t0 + n if last else t0 + n + 1
                    if first:
                        k.memset("pool", ug[gs], ug[gs][:, :, 0:1], 0.0)
                    if last:
                        k.memset("pool", ug[gs], ug[gs][:, :, n + 1:n + 2], 0.0)
                    srcs = [UU.g[g]] + ([] if first else [UU.g[g - 1]]) + ([] if last else [UU.g[g + 1]])
                    k.dma("sp", ug[gs][:, :, (lo - t0 + 1):(hi - t0 + 1)], UU.ap[:, :, lo:hi].rearrange("c p t -> p c t"),
                          reads=srcs, writes=[ug[gs]], chan=k.bchan(ug[gs], "sp"))
                    k.load("sp", bg[gs], bg[gs][:, :, 0:n], BBs.ap[:, :, t0:t0 + n].rearrange("c p t -> p c t"), src=BBs.g[g])

                load4(0)
                for g, (t0, n) in enumerate(groups):
                    gs = g % 2
                    if g + 1 < NG:
                        load4(g + 1)
                    for c in range(4):
                        e = "dve" if c % 2 == 0 else "dve"
                        cvb = cv[c % 2]
                        k.ts(e, cvb, cvb[:, 0:n], ug[gs], ug[gs][:, c, 0:n], cwt[:, l, c, 0:1], None, ALU.mult, rd=[cwt])
                        k.stt(cvb, cvb[:, 0:n], ug[gs], ug[gs][:, c, 1:n + 1], cwt[:, l, c, 1:2], cvb, cvb[:, 0:n], ALU.mult, ALU.add, rd=[cwt])
                        k.stt(cvb, cvb[:, 0:n], ug[gs], ug[gs][:, c, 2:n + 2], cwt[:, l, c, 2:3], cvb, cvb[:, 0:n], ALU.mult, ALU.add, rd=[cwt])
                        k.tt("pool", cT[gs], cT[gs][:, c, 0:n], cvb, cvb[:, 0:n], bg[gs], bg[gs][:, c, 0:n], ALU.mult)
                    for fc in range(8):
                        p = pc_.nxt()
                        for c in range(4):
                            k.mm(p, p[:, 0:n], wco, wco[:, c, fc * 128:(fc + 1) * 128], cT[gs], cT[gs][:, c, 0:n], c == 0, c == 3)
                        k.cp("act", ycs[gs], ycs[gs][:, fc, 0:n], p, p[:, 0:n])
                    k.store("sp", YC.g[g], YC.ap[:, :, t0:t0 + n].rearrange("c p t -> p c t"), ycs[gs], ycs[gs][:, :, 0:n])
                k.barrier()
            if stop_after == ("P4", l):
                break

            with contextlib.ExitStack() as pst:
                k.phase_begin(pst)
                KTh = [k.sb("KTh%d" % i, [96, T], BF16) for i in range(2)]
                QTh = [k.sb("QTh%d" % i, [96, T], BF16) for i in range(2)]
                Vh = [k.sb("Vh%d" % i, [128, NT, 65], BF16) for i in range(2)]
                pT = [k.sb("pT%d" % i, [128, 512], BF16) for i in range(4)]
                osb = [k.sb("osb%d" % i, [65, 512], F32) for i in range(2)]
                rden = k.sb("rden", [64, 512], F32)
                onb = [k.sb("onb%d" % i, [64, 512], BF16) for i in range(2)]
                sel = k.sb("sel", [65, 64], F32)
                k.memset("pool", sel, sel[:], 0.0)
                k.memset("pool", sel, sel[64:65, :], 1.0)
                pss = RR([k.ps("pss%d" % i, [128, 512], F32) for i in range(5)])
                pso = [k.ps("pso%d" % i, [128, 512], F32) for i in range(2)]
                pden = k.ps("pden", [128, 512], F32)
                allg = list(range(NG))
                qgroups = list(range(NG)) if l == 0 else list(range(1, NG))
                SK = 2
                qi = 0
                def load_head(h):
                    hs = h % 2
                    k.dma("sp", KTh[hs][0:64, :], KN.ap[h, :, :], reads=[KN.g[g_] for g_ in allg], writes=[KTh[hs]], chan=_ch(k, KTh[hs]))
                    k.dma("sp", KTh[hs][64:96, :], KR.ap[:, :], reads=[KR.g[g_] for g_ in allg], writes=[KTh[hs]], chan=_ch(k, KTh[hs]))
                    k.dma("sp", QTh[hs][:, :], QT.ap[h, :, :], reads=[QT.g[g_] for g_ in allg], writes=[QTh[hs]], chan=_ch(k, QTh[hs]))
                    k.dma("sp", Vh[hs][:, :, :], VV.ap[h, :, :].rearrange("(t p) e -> p t e", p=128), reads=[VV.g[g_] for g_ in allg],
                          writes=[Vh[hs]], chan=_ch(k, Vh[hs]))

                load_head(0)
                for h in range(8):
                    hs = h % 2
                    pr, hh = h // 2, h % 2
                    if h + 1 < 8:
                        load_head(h + 1)
                    for g in qgroups:
                        q0, nq = groups[g]
                        nkt = 2 if g == 0 else NT
                        po = pso[qi % 2]
                        pSs = {}
                        for kt in range(nkt + SK):
                            if kt < nkt:
                                pS = pss.nxt()
                                k.mm(pS, pS[:, 0:nq], KTh[hs], KTh[hs][:, kt * 128:(kt + 1) * 128], QTh[hs], QTh[hs][:, q0:q0 + nq], True, True)
                                k.act(pT[kt % 4], pT[kt % 4][:, 0:nq], pS, pS[:, 0:nq], AF.Exp)
                            if kt >= SK:
                                kk = kt - SK
                                k.mm(po, po[0:65, 0:nq], Vh[hs], Vh[hs][:, kk, :], pT[kk % 4], pT[kk % 4][:, 0:nq], kk == 0, kk == nkt - 1)
                        ob = osb[qi % 2]
                        k.cp("dve", ob, ob[:, 0:nq], po, po[0:65, 0:nq])
                        k.mm(pden, pden[0:64, 0:nq], sel, sel[:, :], ob, ob[:, 0:nq], True, True)
                        k.op("dve", lambda: nc.vector.reciprocal(out=rden[:, 0:nq], in_=pden[0:64, 0:nq]), reads=[pden], writes=[rden])
                        k.tt("pool", onb[qi % 2], onb[qi % 2][:, 0:nq], ob, ob[0:64, 0:nq], rden, rden[:, 0:nq], ALU.mult)
                        k.store("sp", OT.g[g], OT.ap[pr, hh * 64:(hh + 1) * 64, q0:q0 + nq], onb[qi % 2], onb[qi % 2][:, 0:nq])
                        qi += 1
                k.barrier()
            if stop_after == ("P5", l):
                break

            with contextlib.ExitStack() as pst:
                k.phase_begin(pst)
                wmo = k.sb("wmo", [128, 4, D], BF16)
                k.load("pool", wmo, wmo[:], w_mla_o[l, :, :].rearrange("(c p) f -> p c f", p=128))
                wout = k.sb("wout", [128, 8, D], BF16)
                k.load("pool", wout, wout[:], w_out[l, :, :].rearrange("(c p) f -> p c f", p=128))
                wr = k.sb("wr", [128, 8, NE], F32)
                k.load("sp", wr, wr[:], w_router[:, :].rearrange("(c p) e -> p c e", p=128))
                g1b = [k.sb("g1b%d" % i, [128, D], F32) for i in range(2)]
                for r in range(2):
                    k.load("sp", g1b[r], g1b[r][:], MOD[l, r:r + 1, 2 * D:3 * D].partition_broadcast(128), src=MOD)
                otg = [k.sb("otg%d" % i, [128, 4, 256], BF16) for i in range(2)]
                yrg = [k.sb("yrg%d" % i, [128, 8, 256], BF16) for i in range(2)]
                ycg = [k.sb("ycg%d" % i, [128, 8, 256], BF16) for i in range(2)]
                gtg2 = [[k.sb("gtg%d_%d" % (i, b_), [128, 8, 256], BF16) for b_ in range(3)] for i in range(2)]
                a1 = [k.sb("a1_%d" % i, [128, 512], F32) for i in range(2)]
                a2 = [k.sb("a2_%d" % i, [128, 512], F32) for i in range(2)]
                a3 = [k.sb("a3_%d" % i, [128, 512], F32) for i in range(2)]
                mT = k.sb("mT", [128, 8, 512], BF16)
                xin = [k.sb("xin6_%d" % i, [128, D], F32) for i in range(2)]
                xm = [k.sb("xm%d" % i, [128, D], F32) for i in range(2)]
                tt1_ = [k.sb("tt1_%d" % i, [128, D], F32) for i in range(2)]
                junk = k.sb("junk6", [128, D], BF16)
                st6 = [k.sb("st6_%d" % i, [128, 8], F32) for i in range(2)]
                xn2_ = [k.sb("xn2_%d" % i, [128, D], F32) for i in range(2)]
                h2f_ = [k.sb("h2f_%d" % i, [128, 8, 128], F32) for i in range(2)]
                h2m_ = [k.sb("h2m_%d" % i, [128, 8, 128], F32) for i in range(1)] * 2
                g2nb = [k.sb("g2nb%d" % i, [128, D], F32) for i in range(2)]
                sh2b = [k.sb("sh2b%d" % i, [128, D], F32) for i in range(2)]
                n2b = k.sb("n2b", [128, D], F32)
                k.load("sp", n2b, n2b[:], norm2_r[l, 0:1, :].partition_broadcast(128))
                for r in range(2):
                    k.load("sp", g2nb[r], g2nb[r][:], MOD[l, r:r + 1, 4 * D:5 * D].partition_broadcast(128), src=MOD)
                    k.load("sp", sh2b[r], sh2b[r][:], MOD[l, r:r + 1, 3 * D:4 * D].partition_broadcast(128), src=MOD)
                    k.stt(g2nb[r], g2nb[r][:], g2nb[r], g2nb[r][:], 1.0, n2b, n2b[:], ALU.add, ALU.mult)
                h2t_ = tt1_
                h2tb_ = [k.sb("h2tb%d" % i, [128, D], BF16) for i in range(2)]
                gslb_ = [k.sb("gslb%d" % i, [128, 4], BF16) for i in range(2)]
                k.memset("dve", carry, carry[:], 0.0)
                rt_ = [k.sb("rt%d" % i, [128, 16, 16], F32) for i in range(2)]
                gto = [k.sb("gto%d" % i, [128, NE], F32) for i in range(2)]
                pm = RR([k.ps("pm6_%d" % i, [128, 512], F32) for i in range(3)])
                pox_ = [k.ps("pox%d" % i, [128, D], F32) for i in range(1)] * 2
                ptf = k.ps("ptf", [128, 8, 128], F32)
                plg_ = k.ps("plg", [128, 2, 32], F32)
                ti = 0
                units6 = []
                for g in (range(NG) if l == 0 else range(1, NG)):
                    t0, n = groups[g]
                    if n == 512:
                        units6 += [(g, t0, 256), (g, t0 + 256, 256)]
                    else:
                        units6.append((g, t0, n))

                def load6(u):
                    g, t0, n = units6[u]
                    us = u % 2
                    k.load("sp", otg[us], otg[us][:, :, 0:n], OT.ap[:, :, t0:t0 + n].rearrange("c p t -> p c t"), src=OT.g[g])
                    k.load("sp", yrg[us], yrg[us][:, :, 0:n], YR.ap[:, :, t0:t0 + n].rearrange("c p t -> p c t"), src=YR.g[g])
                    k.load("sp", ycg[us], ycg[us][:, :, 0:n], YC.ap[:, :, t0:t0 + n].rearrange("c p t -> p c t"), src=YC.g[g])
                    for b_ in range(3):
                        k.load("sp", gtg2[us][b_], gtg2[us][b_][:, :, 0:n], GT.ap[b_ * 8:(b_ + 1) * 8, :, t0:t0 + n].rearrange("c p t -> p c t"), src=GT.g[g])

                load6(0)
                for u, (g, t0, n) in enumerate(units6):
                    gs = u % 2
                    gtg = gtg2[gs]
                    r = 1 if g == 0 else 0
                    ntile = n // 128
                    if u + 1 < len(units6):
                        load6(u + 1)
                    for fc in range(8):
                        fs = fc % 2
                        p = pm.nxt()
                        for pr in range(4):
                            k.mm(p, p[:, 0:n], wmo, wmo[:, pr, fc * 128:(fc + 1) * 128], otg[gs], otg[gs][:, pr, 0:n], pr == 0, pr == 3)
                        k.tt("dve", a3[fs], a3[fs][:, 0:n], p, p[:, 0:n], gtg[2], gtg[2][:, fc, 0:n], ALU.mult)
                        k.tt("dve", a1[fs], a1[fs][:, 0:n], yrg[gs], yrg[gs][:, fc, 0:n], gtg[0], gtg[0][:, fc, 0:n], ALU.mult)
                        k.tt("pool", a2[fs], a2[fs][:, 0:n], ycg[gs], ycg[gs][:, fc, 0:n], gtg[1], gtg[1][:, fc, 0:n], ALU.mult)
                        k.tt("pool", a1[fs], a1[fs][:, 0:n], a1[fs], a1[fs][:, 0:n], a2[fs], a2[fs][:, 0:n], ALU.add)
                        k.tt("dve", mT, mT[:, fc, 0:n], a1[fs], a1[fs][:, 0:n], a3[fs], a3[fs][:, 0:n], ALU.add)
                    tbase = ti
                    ti += ntile

                    def t_out(j):
                        xs = (tbase + j) % 2
                        cs = slice(j * 128, (j + 1) * 128)
                        pox = pox_[xs]
                        for half in range(2):
                            for kc in range(8):
                                k.mm(pox, pox[:, half * 512:(half + 1) * 512], mT, mT[:, kc, cs], wout, wout[:, kc, half * 512:(half + 1) * 512], kc == 0, kc == 7)

                    def t_mid(j):
                        t = t0 // 128 + j
                        xs = (tbase + j) % 2
                        tt1 = tt1_[xs]; xn2 = xn2_[xs]; pox = pox_[xs]; st = st6[xs]
                        sap, sbuf_ = res_src(l, t)
                        k.load("sp", xin[xs], xin[xs][:], sap, src=sbuf_)
                        k.tt("dve", tt1, tt1[:], pox, pox[:], g1b[r], g1b[r][:], ALU.mult)
                        k.tt("dve", xm[xs], xm[xs][:], tt1, tt1[:], xin[xs], xin[xs][:], ALU.add)
                        k.store("sp", XM.g[g], XM.ap[t * 128:(t + 1) * 128, :], xm[xs], xm[xs][:])
                        k.act(junk, junk[:], xm[xs], xm[xs][:], AF.Square, accb=st, accum_out=st[:, 0:1])
                        k.act(st, st[:, 1:2], st, st[:, 0:1], AF.Ln, scale=1.0 / D, bias=EPS)
                        k.act(st, st[:, 2:3], st, st[:, 1:2], AF.Exp, scale=-0.5)
                        k.ts("dve", xn2, xn2[:], xm[xs], xm[xs][:], st[:, 2:3], None, ALU.mult, rd=[st])

                    def t_tr(j):
                        t = t0 // 128 + j
                        xs = (tbase + j) % 2
                        xn2 = xn2_[xs]; h2f = h2f_[xs]
                        for c in range(8):
                            k.tr(ptf, ptf[:, c, :], xn2, xn2[:, c * 128:(c + 1) * 128], ident32, ident32[:], inc=(c == 7))
                        for c in range(8):
                            k.act(h2f, h2f[:, c, :], ptf, ptf[:, c, :], AF.Identity, rd=[vecs],
                                  scale=vecs[:, l, r, 2, c:c + 1], bias=vecs[:, l, r, 3, c:c + 1])
                        h2t = h2t_[xs]; h2tb = h2tb_[xs]
                        k.tt("dve", h2t, h2t[:], xn2, xn2[:], g2nb[r], g2nb[r][:], ALU.mult)
                        k.tt("pool", h2tb, h2tb[:], h2t, h2t[:], sh2b[r], sh2b[r][:], ALU.add)
                        k.store("sp", H2R.g[g], H2R.ap[t * 128:(t + 1) * 128, :], h2tb, h2tb[:])

                    def t_rt_ops(j):
                        ops = []
                        tail = []
                        t = t0 // 128 + j
                        xs = (tbase + j) % 2
                        h2f = h2f_[xs]; rt = rt_[xs]; gslb = gslb_[xs]
                        plg = plg_
                        for kc in range(8):
                            k.mm(plg, plg[:, xs, 0:16], h2f, h2f[:, kc, :], wr, wr[:, kc, :], kc == 0, kc == 7)
                        sc_ = rt[:, 0, :]; bs = rt[:, 1, :]; eq1 = rt[:, 2, :]; bs2 = rt[:, 3, :]; eq2 = rt[:, 4, :]
                        m1 = rt[:, 5, 0:4]; m2 = rt[:, 5, 4:8]; gsm = rt[:, 5, 8:12]; gmx = rt[:, 5, 12:13]; wsm = rt[:, 5, 13:14]; rws = rt[:, 5, 14:15]
                        gsl = rt[:, 6, 0:4]; msk = rt[:, 7, :]; wgt = rt[:, 8, :]; ex = rt[:, 10, :]
                        v3 = lambda ap: ap.rearrange("p (g e) -> p g e", g=4)
                        bc4 = lambda ap: ap.unsqueeze(2).to_broadcast([128, 4, 4])
                        ops.append(lambda: k.act(rt, ex, plg, plg[:, xs, 0:16], AF.Exp, scale=-1.0))
                        ops.append(lambda: k.ts("dve", rt, ex, rt, ex, 1.0, None, ALU.add))
                        ops.append(lambda: k.op("dve", lambda: nc.vector.reciprocal(out=sc_, in_=ex), reads=[rt], writes=[rt]))
                        ops.append(lambda: k.tt("dve", rt, bs, rt, sc_, rbias, rbias[:], ALU.add))
                        ops.append(lambda: k.op("dve", lambda: nc.vector.tensor_reduce(out=m1, in_=v3(bs), axis=AX.X, op=ALU.max), reads=[rt], writes=[rt]))
                        ops.append(lambda: k.tt("dve", rt, v3(eq1), rt, v3(bs), rt, bc4(m1), ALU.is_equal))
                        ops.append(lambda: k.stt(rt, bs2, rt, eq1, -1.0e9, rt, bs, ALU.mult, ALU.add))
                        ops.append(lambda: k.op("dve", lambda: nc.vector.tensor_reduce(out=m2, in_=v3(bs2), axis=AX.X, op=ALU.max), reads=[rt], writes=[rt]))
                        ops.append(lambda: k.tt("dve", rt, gsm, rt, m1, rt, m2, ALU.add))
                        ops.append(lambda: k.op("dve", lambda: nc.vector.tensor_reduce(out=gmx, in_=gsm, axis=AX.X, op=ALU.max), reads=[rt], writes=[rt]))
                        ops.append(lambda: k.ts("dve", rt, gsl, rt, gsm, gmx, None, ALU.is_equal))
                        ops.append(lambda: k.tt("dve", rt, v3(eq2), rt, v3(bs2), rt, bc4(m2), ALU.is_equal))
                        ops.append(lambda: k.tt("dve", rt, msk, rt, eq1, rt, eq2, ALU.add))
                        ops.append(lambda: k.tt("dve", rt, v3(msk), rt, v3(msk), rt, bc4(gsl), ALU.mult))
                        ops.append(lambda: k.tt("dve", rt, wgt, rt, sc_, rt, msk, ALU.mult))
                        ops.append(lambda: k.op("dve", lambda: nc.vector.tensor_reduce(out=wsm, in_=wgt, axis=AX.X, op=ALU.add), reads=[rt], writes=[rt]))
                        ops.append(lambda: k.op("dve", lambda: nc.vector.reciprocal(out=rws, in_=wsm), reads=[rt], writes=[rt]))
                        ops.append(lambda: k.ts("dve", gto[xs], gto[xs][:], rt, wgt, rws, None, ALU.mult, rd=[rt]))
                        ops.append(lambda: k.op("dve", lambda: nc.vector.tensor_reduce(out=G16A[:, t, 0:4], in_=gto[xs][:].rearrange("p (g e) -> p e g", g=4), axis=AX.X, op=ALU.add),
                             reads=[gto[xs]], writes=[G16A]))
                        ops.append(lambda: k.cp("dve", GSLA, GSLA[:, t, :], rt, gsl))
                        ops.append(lambda: k.cp("dve", gslb, gslb[:], rt, gsl))
                        tail.append(lambda: k.mm(plg, plg[:, xs, 16:20], ustrb, ustrb[:], gslb, gslb[:], True, True, inc=False))
                        tail.append(lambda: k.mm(plg, plg[:, xs, 20:24], onesb, onesb[:], gslb, gslb[:], True, True))
                        rkf = rt[:, 9, 0:4]; rkm = rt[:, 9, 4:8]
                        tail.append(lambda: k.tt("dve", rt, rkf, plg, plg[:, xs, 16:20], carry, carry[:], ALU.add))
                        tail.append(lambda: k.tt("dve", rt, rkm, rt, rkf, rt, gsl, ALU.mult))
                        tail.append(lambda: k.op("dve", lambda: nc.vector.tensor_reduce(out=RANKA[:, t:t + 1], in_=rkm, axis=AX.X, op=ALU.add), reads=[rt], writes=[RANKA]))
                        tail.append(lambda: k.tt("dve", carry, carry[:], carry, carry[:], plg, plg[:, xs, 20:24], ALU.add))
                        return ops, tail

                    seq = []
                    for j in range(ntile + 3):
                        if j < ntile:
                            seq.append((t_out, j)); seq.append((t_mid, j))
                        if 1 <= j <= ntile:
                            seq.append((t_tr, j - 1))
                        if 2 <= j <= ntile + 1:
                            seq.append(("rt", j - 2))
                    pend = []
                    for fn_, j_ in seq:
                        if fn_ == "rt":
                            pend.append(t_rt_ops(j_))
                            if len(pend) == 2 or j_ == ntile - 1:
                                n_ops = max(len(p_[0]) for p_ in pend)
                                for oi_ in range(n_ops):
                                    for p_ in pend:
                                        if oi_ < len(p_[0]):
                                            p_[0][oi_]()
                                for p_ in pend:
                                    for f_ in p_[1]:
                                        f_()
                                pend = []
                        else:
                            fn_(j_)
                k.barrier()
            if stop_after == ("P6", l):
                break

            tiles_l = list(range(NT)) if l == 0 else list(range(2, NT))
            with contextlib.ExitStack() as pst:
                k.phase_begin(pst)
                t49 = k.sb("t49", [128, 4, 9], F32)
                nbv = k.sb("nbv", [128, 4], F32)
                base = k.sb("base", [128, 4], F32)
                gbf = k.sb("gbf", [128, NB], F32)
                gbt = k.sb("gbt", [128, NB], F32)
                idxf = k.sb("idxf", [128, NB, 4], F32)
                slt = k.sb("slt", [128, NT, 4], F32)
                slf = k.sb("slf", [128, NT], F32)
                zt16 = k.sb("zt16", [128, 8 * D], BF16)
                zg = k.sb("zg", [128, (NS // 128) * 16], F32)
                hb2 = [k.sb("hb2_%d" % i, [128, D], BF16) for i in range(6)]
                k.tt("dve", t49, t49[:], carry, carry[:, :].unsqueeze(2).to_broadcast([128, 4, 9]),
                     mc, mc[:, 0:9].unsqueeze(1).to_broadcast([128, 4, 9]), ALU.is_gt)
                k.op("dve", lambda: nc.vector.tensor_reduce(out=nbv[:], in_=t49[:], axis=AX.X, op=ALU.add), reads=[t49], writes=[nbv])
                k.memset("dve", base, base[:, 0:1], 0.0)
                for g_ in range(1, 4):
                    k.stt(base, base[:, g_:g_ + 1], nbv, nbv[:, g_ - 1:g_], 1024.0, base, base[:, g_ - 1:g_], ALU.mult, ALU.add)
                k.memset("dve", gbf, gbf[:], 0.0)
                for g_ in range(1, 4):
                    k.ts("dve", gbt, gbt[:], mc, mc[:, 16:16 + NB], base[:, g_:g_ + 1], None, ALU.is_ge, rd=[base])
                    k.tt("dve", gbf, gbf[:], gbf, gbf[:], gbt, gbt[:], ALU.add)
                k.stt(idxf, idxf[:], gbf, gbf[:, :].unsqueeze(2).to_broadcast([128, NB, 4]), 512.0,
                      mc, mc[:, 40:44].unsqueeze(1).to_broadcast([128, NB, 4]), ALU.mult, ALU.add)
                if l > 0:
                    k.ts("dve", idxf, idxf[:], idxf, idxf[:], float(l * NE * 128), None, ALU.add)
                k.cp("dve", IDXW, IDXW[:], idxf, idxf[:])
                k.tt("dve", slt, slt[:], GSLA, GSLA[:], base, base[:, :].unsqueeze(1).to_broadcast([128, NT, 4]), ALU.mult)
                k.op("dve", lambda: nc.vector.tensor_reduce(out=slf[:], in_=slt[:], axis=AX.X, op=ALU.add), reads=[slt], writes=[slf])
                k.tt("dve", slf, slf[:], slf, slf[:], RANKA, RANKA[:], ALU.add)
                k.cp("dve", SLOTI, SLOTI[:], slf, slf[:])
                k.memset("pool", zt16, zt16[:], 0.0)
                k.memset("pool", zg, zg[:], 0.0)
                zfill = Buf("zfill")
                for b_ in range(NB):
                    k.dma("sp", H2S[b_ * 1024:(b_ + 1) * 1024, :].rearrange("(p j) f -> p (j f)", p=128), zt16[:], reads=[zt16], writes=[zfill],
                          chan=k.bchan(zt16, "sp"))
                k.dma("sp", G4S[:, :].rearrange("(p j) e -> p (j e)", p=128), zg[:], reads=[zg], writes=[zfill], chan=k.bchan(zg, "sp"))
                for i_, t in enumerate(tiles_l):
                    hb = hb2[i_ % 6]
                    k.load("sp", hb, hb[:], H2R.ap[t * 128:(t + 1) * 128, :], src=H2R.g[gi_of_tile(t)])
                    k.dma("pool", H2S[:, :], hb[:], reads=[hb, SLOTI, zfill], writes=[], chan=k.bchan(hb, "pool"), idx=("scatter", SLOTI[:, t:t + 1]))
                    k.dma("pool", G4S[:, :], G16A[:, t, :], reads=[G16A, SLOTI, zfill], writes=[], chan=k.bchan(G16A, "pool"), idx=("scatter", SLOTI[:, t:t + 1]))
                k.barrier()
            if stop_after == ("P6b", l):
                break

            with contextlib.ExitStack() as pst:
                k.phase_begin(pst)
                NBL = (len(tiles_l) * 128 + 4 * 1023) // 1024
                assert NBL <= NB
                w1e = [k.sb("w1e%d" % i, [128, 8, 512], BF16) for i in range(2)]
                w3e = [k.sb("w3e%d" % i, [128, 8, 512], BF16) for i in range(2)]
                w2e = [k.sb("w2e%d" % i, [128, 4, D], BF16) for i in range(2)]
                hblk = [k.sb("hblk%d" % i, [128, 8, D], BF16) for i in range(2)]
                g4b = [k.sb("g4b%d" % i, [128, 8, 16], F32) for i in range(2)]
                h2T = [k.sb("h2T%d" % i, [128, 8, 1024], BF16) for i in range(2)]
                acc = k.sb("acc", [128, 8, D], F32)
                sl = [k.sb("sl%d" % i, [128, 512], F32) for i in range(2)]
                zT = [k.sb("zT%d" % i, [128, 4, 512], BF16) for i in range(2)]
                pab = RR([k.ps("pab%d" % i, [128, 512], F32) for i in range(4)])
                ph = RR([k.ps("ph%d" % i, [128, 512], F32) for i in range(3)])
                ptr = k.ps("ptr7", [128, 8, 128], BF16)

                def load_w(b_, ee, which):
                    es = (b_ * 4 + ee) % 2
                    ix = IDXW[:, b_, ee:ee + 1]
                    if which == 0:
                        k.dma("pool", w1e[es][:].rearrange("p a b -> p (a b)"), w1r[:, :, :].rearrange("l r c -> (l r) c"), reads=[IDXW], writes=[w1e[es]],
                              chan=k.bchan(w1e[es], "pool"), idx=("gather", ix))
                        k.dma("pool", w3e[es][:].rearrange("p a b -> p (a b)"), w3r[:, :, :].rearrange("l r c -> (l r) c"), reads=[IDXW], writes=[w3e[es]],
                              chan=k.bchan(w3e[es], "pool"), idx=("gather", ix))
                    else:
                        k.dma("pool", w2e[es][:].rearrange("p a b -> p (a b)"), w2r[:, :, :].rearrange("l r c -> (l r) c"), reads=[IDXW], writes=[w2e[es]],
                              chan=k.bchan(w2e[es], "pool"), idx=("gather", ix))

                def load_blk(b_):
                    bs = b_ % 2
                    k.load("sp", hblk[bs], hblk[bs][:], H2S[b_ * 1024:(b_ + 1) * 1024, :].rearrange("(j p) f -> p j f", p=128))
                    k.load("sp", g4b[bs], g4b[bs][:], G4S[b_ * 1024:(b_ + 1) * 1024, :].rearrange("(j p) e -> p j e", p=128))

                def transp_blk(b_):
                    bs = b_ % 2
                    for j in range(8):
                        for c in range(8):
                            k.tr(ptr, ptr[:, c, :], hblk[bs], hblk[bs][:, j, c * 128:(c + 1) * 128], identb, identb[:], inc=(c == 7))
                        k.cp("act" if j % 2 == 0 else "dve", h2T[bs], h2T[bs][:, :, j * 128:(j + 1) * 128], ptr, ptr[:])

                units = [(b_, ee, half) for b_ in range(NBL) for ee in range(4) for half in range(2)]

                def stage_a(ui):
                    b_, ee, half = units[ui]
                    bs = b_ % 2
                    es = (b_ * 4 + ee) % 2
                    zs = ui % 2
                    c0 = half * 512
                    if half == 0:
                        if ee == 0 and b_ == 0:
                            load_blk(0)
                            load_w(0, 0, 0)
                            load_w(0, 0, 1)
                            transp_blk(0)
                        nb_, ne_ = (b_, ee + 1) if ee < 3 else (b_ + 1, 0)
                        if nb_ < NBL:
                            load_w(nb_, ne_, 0)
                        if ee == 1 and b_ + 1 < NBL:
                            load_blk(b_ + 1)
                    for jc in range(4):
                        pa = pab.nxt()
                        for kc in range(8):
                            k.mm(pa, pa[:, :], w1e[es], w1e[es][:, kc, jc * 128:(jc + 1) * 128], h2T[bs], h2T[bs][:, kc, c0:c0 + 512], kc == 0, kc == 7)
                        pb = pab.nxt()
                        for kc in range(8):
                            k.mm(pb, pb[:, :], w3e[es], w3e[es][:, kc, jc * 128:(jc + 1) * 128], h2T[bs], h2T[bs][:, kc, c0:c0 + 512], kc == 0, kc == 7)
                        k.act(sl[jc % 2], sl[jc % 2][:], pa, pa[:, :], AF.Silu)
                        k.tt("dve", zT[zs], zT[zs][:, jc, :], sl[jc % 2], sl[jc % 2][:], pb, pb[:, :], ALU.mult)

                def stage_h(ui):
                    b_, ee, half = units[ui]
                    bs = b_ % 2
                    es = (b_ * 4 + ee) % 2
                    zs = ui % 2
                    for j in range(4):
                        tj = half * 4 + j
                        for hf in range(2):
                            pq = ph.nxt()
                            for jc in range(4):
                                k.mm(pq, pq[:, :], zT[zs], zT[zs][:, jc, j * 128:(j + 1) * 128],
                                     w2e[es], w2e[es][:, jc, hf * 512:(hf + 1) * 512], jc == 0, jc == 3)
                            oa = acc[:, tj, hf * 512:(hf + 1) * 512]
                            if ee == 0:
                                k.ts("dve", acc, oa, pq, pq[:, :], g4b[bs][:, tj, 0:1], None, ALU.mult, rd=[g4b[bs]])
                            else:
                                k.stt(acc, oa, pq, pq[:, :], g4b[bs][:, tj, ee:ee + 1], acc, oa, ALU.mult, ALU.add, rd=[g4b[bs]])
                    if ee == 3 and half == 1:
                        k.store("sp", YS, YS[b_ * 1024:(b_ + 1) * 1024, :].rearrange("(j p) f -> p j f", p=128), acc, acc[:])

                for ui in range(len(units) + 1):
                    if ui < len(units):
                        stage_a(ui)
                    if ui >= 1:
                        stage_h(ui - 1)
                    if ui < len(units):
                        b_, ee, half = units[ui]
                        if half == 0:
                            nb_, ne_ = (b_, ee + 1) if ee < 3 else (b_ + 1, 0)
                            if nb_ < NBL:
                                load_w(nb_, ne_, 1)
                        if ee == 3 and half == 0 and b_ + 1 < NBL:
                            transp_blk(b_ + 1)
                k.barrier()
            if stop_after == ("P7s", l):
                break

            with contextlib.ExitStack() as pst:
                k.phase_begin(pst)
                g2b = [k.sb("g2b%d" % i, [128, D], F32) for i in range(2)]
                for r in range(2):
                    k.load("sp", g2b[r], g2b[r][:], MOD[l, r:r + 1, 5 * D:6 * D].partition_broadcast(128), src=MOD)
                fnb = k.sb("fnb", [128, D], F32)
                k.load("sp", fnb, fnb[:], final_norm[0:1, :].partition_broadcast(128))
                NS8 = 6
                yg = [k.sb("yg%d" % i, [128, D], F32) for i in range(NS8)]
                xmt = [k.sb("xmt%d" % i, [128, D], F32) for i in range(NS8)]
                xo = [k.sb("xo%d" % i, [128, D], F32) for i in range(3)]
                junk = k.sb("junk8", [128, D], BF16)
                st8 = [k.sb("st8_%d" % i, [128, 8], F32) for i in range(3)]

                def fetch8(i_):
                    t = tiles_l[i_]
                    xs = i_ % NS8
                    k.dma("pool", yg[xs][:], YS[:, :], reads=[SLOTI, YS], writes=[yg[xs]], chan=k.bchan(yg[xs], "pool"), idx=("gather", SLOTI[:, t:t + 1]))
                    k.load("sp", xmt[xs], xmt[xs][:], XM.ap[t * 128:(t + 1) * 128, :], src=XM.g[gi_of_tile(t)])

                AH = 4
                for i_ in range(min(AH, len(tiles_l))):
                    fetch8(i_)
                for i_, t in enumerate(tiles_l):
                    xs = i_ % NS8
                    x3 = i_ % 3
                    g = gi_of_tile(t)
                    r = 1 if t < 2 else 0
                    if i_ + AH < len(tiles_l):
                        fetch8(i_ + AH)
                    k.tt("dve", xo[x3], xo[x3][:], yg[xs], yg[xs][:], g2b[r], g2b[r][:], ALU.mult)
                    k.tt("dve", xo[x3], xo[x3][:], xo[x3], xo[x3][:], xmt[xs], xmt[xs][:], ALU.add)
                    if l == 0:
                        k.store("sp", XR.g[g], XR.ap[t * 128:(t + 1) * 128, :], xo[x3], xo[x3][:])
                    else:
                        st = st8[x3]
                        k.act(junk, junk[:], xo[x3], xo[x3][:], AF.Square, accb=st, accum_out=st[:, 0:1])
                        k.act(st, st[:, 1:2], st, st[:, 0:1], AF.Ln, scale=1.0 / D, bias=EPS)
                        k.act(st, st[:, 2:3], st, st[:, 1:2], AF.Exp, scale=-0.5)
                        k.stt(xo[x3], xo[x3][:], xo[x3], xo[x3][:], st[:, 2:3], fnb, fnb[:], ALU.mult, ALU.mult, rd=[st])
                        k.store("sp", ybuf, y_out[(t - 2) * 128:(t - 1) * 128, :], xo[x3], xo[x3][:])
                k.barrier()
        k.barrier()
        final = {k.sem[e]: k.cnt[e] for e in k.ENGS}
        for c in k.chans + k.swchans + k.extra_chans:
            final[c.sem] = c.cnt
        for sem_, v in k.maxwait.items():
            assert v <= final[sem_], ("wait on never-reached semaphore value", sem_, v, final[sem_])
    return nc


def _ch(k, b):
    return k.bchan(b, "sp")


_CACHE = {}


def make_in_maps(inputs):
    x = np.asarray(inputs["x"], np.float32)
    B, S, _ = x.shape
    NX = S // 128
    hw = host_weights({k_: np.asarray(v, np.float32) for k_, v in inputs.items()})
    tabs = host_tables(NX)
    shared = dict(hw)
    shared.update(tabs)
    shared["ident"] = np.eye(128, dtype=np.float32)
    ctx = np.asarray(inputs["ctx"], np.float32)
    c = np.asarray(inputs["c"], np.float32)
    in_maps = []
    for b in range(B):
        m = dict(shared)
        m["x"] = np.ascontiguousarray(x[b])
        m["ctx"] = np.ascontiguousarray(ctx[b])
        m["c_t"] = _fm(c[b], 8)
        in_maps.append(m)
    return NX, in_maps


def kernel(**inputs):
    NX, in_maps = make_in_maps(inputs)
    B = len(in_maps)
    nc = build(NX)
    res = run_bass_kernel_spmd(nc, in_maps, core_ids=list(range(B)))
    return np.stack([np.asarray(r["y"], np.float32) for r in res.results], 0)
```
